# Optimizing a Trainium2 kernel written in Bass

```python
import math, functools
import jax, jax.numpy as jnp
from jax import lax
import numpy as np

D_MODEL = 1024
BATCH = 2
SEQ = 8192
DEPTH = 1

GRID_W = 64
CTX_LEN = 256
HEAD_DIM = 64
A_HEADS = 8
A_KV_HEADS = 2
A_GROUP = A_HEADS // A_KV_HEADS
A_WIDTH = A_HEADS * HEAD_DIM
B_HEADS = 4
B_V_DIM = 2 * HEAD_DIM
B_WIDTH = B_HEADS * B_V_DIM
N_BRANCH = 2
A_Q_COLS = A_HEADS * HEAD_DIM
A_KV_COLS = A_KV_HEADS * HEAD_DIM
B_QK_COLS = B_HEADS * 2 * HEAD_DIM
IN_SPLIT = [A_Q_COLS, A_KV_COLS, A_KV_COLS, B_QK_COLS, B_QK_COLS, B_WIDTH, D_MODEL, D_MODEL]
IN_COLS = sum(IN_SPLIT)
IN_OFFSETS = [int(v) for v in np.cumsum(IN_SPLIT)[:-1]]
ROPE_THETA = 10000.0
AXIS_PAIRS = HEAD_DIM // 4
Q_BLOCK = 128
N_EXPERTS = 32
TOP_K = 4
D_EXPERT = D_MODEL
SWIGLU_LIMIT = 7.0
SWIGLU_ALPHA = 1.702
MOE_BLOCK = 128
N_MOD = 6
EPS = 1e-6
SUBLN_EPS = 1e-5

kernel_name = "hybrid_gqa_diffattn_moe_dit_block"


def _rmsnorm(x, gain, eps=EPS):
    xf = x.astype(jnp.float32)
    y = xf * lax.rsqrt(jnp.mean(xf * xf, axis=-1, keepdims=True) + eps)
    return (y * gain.astype(jnp.float32)).astype(x.dtype)


def _modulate(h, shift, scale):
    return h * (1 + scale) + shift


def _axial_rope(rows):
    row = jnp.repeat(jnp.arange(rows, dtype=jnp.float32), GRID_W)
    col = jnp.tile(jnp.arange(GRID_W, dtype=jnp.float32), rows)
    inv = 1.0 / (ROPE_THETA ** (jnp.arange(AXIS_PAIRS, dtype=jnp.float32) / AXIS_PAIRS))
    ang = jnp.concatenate([row[:, None] * inv, col[:, None] * inv], axis=-1)
    return jnp.cos(ang), jnp.sin(ang)


def _apply_rope(x, cos, sin):
    extra = x.ndim - 3
    cos = cos.reshape(cos.shape[0], *([1] * extra), cos.shape[1])
    sin = sin.reshape(sin.shape[0], *([1] * extra), sin.shape[1])
    xp = x.reshape(*x.shape[:-1], HEAD_DIM // 2, 2)
    x0, x1 = xp[..., 0], xp[..., 1]
    out = jnp.stack([x0 * cos - x1 * sin, x0 * sin + x1 * cos], axis=-1)
    return out.reshape(x.shape).astype(x.dtype)


def _project(h, w_in, q_norm_a, k_norm_a, q_norm_b, k_norm_b):
    b, n, _ = h.shape
    qa, ka, va, qb, kb, vb, ga, gb = jnp.split(h @ w_in, IN_OFFSETS, axis=-1)
    qa = _rmsnorm(qa.reshape(b, n, A_HEADS, HEAD_DIM), q_norm_a)
    ka = _rmsnorm(ka.reshape(b, n, A_KV_HEADS, HEAD_DIM), k_norm_a)
    va = va.reshape(b, n, A_KV_HEADS, HEAD_DIM)
    qb = _rmsnorm(qb.reshape(b, n, B_HEADS, 2, HEAD_DIM), q_norm_b)
    kb = _rmsnorm(kb.reshape(b, n, B_HEADS, 2, HEAD_DIM), k_norm_b)
    vb = vb.reshape(b, n, B_HEADS, B_V_DIM)
    return qa, ka, va, qb, kb, vb, ga, gb


def _gqa_attend(q, k, v):
    b, n = q.shape[:2]
    qg = q.reshape(b, n, A_KV_HEADS, A_GROUP, HEAD_DIM)
    s = jnp.einsum('bqhgd,bkhd->bhgqk', qg, k).astype(jnp.float32) * (HEAD_DIM ** -0.5)
    p = jax.nn.softmax(s, axis=-1).astype(v.dtype)
    o = jnp.einsum('bhgqk,bkhd->bqhgd', p, v)
    return o.reshape(b, n, A_WIDTH)


def _diff_attend(q, k, v, lam):
    s = jnp.einsum('bqhmd,bkhmd->bhmqk', q, k).astype(jnp.float32) * (HEAD_DIM ** -0.5)
    p = jax.nn.softmax(s, axis=-1)
    a = (p[:, :, 0] - lam * p[:, :, 1]).astype(v.dtype)
    return jnp.einsum('bhqk,bkhe->bqhe', a, v)


def _sweep_query_blocks(attend, q, k, v):
    b, s = q.shape[:2]
    nb = s // Q_BLOCK
    qb = jnp.moveaxis(q.reshape(b, nb, Q_BLOCK, *q.shape[2:]), 1, 0)
    o = lax.map(lambda qq: attend(qq, k, v), qb)
    o = jnp.moveaxis(o, 0, 1)
    return o.reshape(b, s, *o.shape[3:])


def _diff_out(o, subln, lam_init):
    o = _rmsnorm(o, subln, SUBLN_EPS) * (1.0 - lam_init)
    return o.reshape(o.shape[0], o.shape[1], B_WIDTH)


def _merge(ya, yb, ga, gb, w_oa, w_ob, w_out):
    merged = jax.nn.sigmoid(ga) * (ya @ w_oa) + jax.nn.sigmoid(gb) * (yb @ w_ob)
    return merged @ w_out


def _moe(h, router_w, router_b, w_gate, b_gate, w_up, b_up, w_down, b_down):
    t = h.shape[0]
    logits = (h @ router_w + router_b).astype(jnp.float32)
    top_logit, top_e = lax.top_k(logits, TOP_K)
    top_w = jax.nn.softmax(top_logit, axis=-1)
    n_assign = t * TOP_K
    flat_e = top_e.reshape(-1)
    order = jnp.argsort(flat_e)
    sorted_e = flat_e[order]
    sorted_tok = order // TOP_K
    sorted_w = top_w.reshape(-1)[order]
    counts = jnp.bincount(flat_e, length=N_EXPERTS)
    padded = (counts + MOE_BLOCK - 1) // MOE_BLOCK * MOE_BLOCK
    pad_end = jnp.cumsum(padded)
    pad_start = pad_end - padded
    start = jnp.cumsum(counts) - counts
    dest = pad_start[sorted_e] + jnp.arange(n_assign) - start[sorted_e]
    n_blocks = -(-n_assign // MOE_BLOCK) + N_EXPERTS
    buf_tok = jnp.zeros((n_blocks * MOE_BLOCK,), jnp.int32).at[dest].set(sorted_tok.astype(jnp.int32))
    block_e = jnp.minimum(jnp.searchsorted(pad_end, jnp.arange(n_blocks) * MOE_BLOCK, side='right'),
                          N_EXPERTS - 1)
    xb = h[buf_tok].reshape(n_blocks, MOE_BLOCK, h.shape[-1])

    def expert_block(args):
        xe, e = args
        gate = jnp.minimum(xe @ w_gate[e] + b_gate[e], SWIGLU_LIMIT)
        up = jnp.clip(xe @ w_up[e] + b_up[e], -SWIGLU_LIMIT, SWIGLU_LIMIT)
        act = (up + 1) * (gate * jax.nn.sigmoid(SWIGLU_ALPHA * gate))
        return act @ w_down[e] + b_down[e]

    yb = lax.map(expert_block, (xb, block_e)).reshape(-1, h.shape[-1])
    y = yb[dest] * sorted_w[:, None].astype(h.dtype)
    return jax.ops.segment_sum(y, sorted_tok, num_segments=t)


def setup_inputs(seed: int = 0) -> dict:
    key = jax.random.key(seed)
    ks = jax.random.split(key, 32)
    f32 = jnp.float32
    L = DEPTH

    def nrm(k, shape, scale):
        return jax.random.normal(k, shape, f32) * scale

    def gain(k, shape):
        return 1.0 + 0.05 * jax.random.normal(k, shape, f32)

    return {
        "x": nrm(ks[0], (BATCH, SEQ, D_MODEL), 1.0),
        "c": nrm(ks[1], (BATCH, D_MODEL), 1.0),
        "ctx": nrm(ks[2], (BATCH, CTX_LEN, D_MODEL), 1.0),
        "c_ctx": nrm(ks[3], (D_MODEL,), 1.0),
        "w_ada": nrm(ks[4], (L, D_MODEL, N_MOD * D_MODEL), 0.5 * D_MODEL ** -0.5),
        "b_ada": nrm(ks[5], (L, N_MOD * D_MODEL), 0.02),
        "norm_attn": gain(ks[6], (L, D_MODEL)),
        "w_in": nrm(ks[7], (L, D_MODEL, IN_COLS), D_MODEL ** -0.5),
        "q_norm_a": gain(ks[8], (L, HEAD_DIM)),
        "k_norm_a": gain(ks[9], (L, HEAD_DIM)),
        "q_norm_b": gain(ks[10], (L, HEAD_DIM)),
        "k_norm_b": gain(ks[11], (L, HEAD_DIM)),
        "lambda_q1": nrm(ks[12], (L, HEAD_DIM), 0.1),
        "lambda_k1": nrm(ks[13], (L, HEAD_DIM), 0.1),
        "lambda_q2": nrm(ks[14], (L, HEAD_DIM), 0.1),
        "lambda_k2": nrm(ks[15], (L, HEAD_DIM), 0.1),
        "subln_b": gain(ks[16], (L, B_V_DIM)),
        "w_oa": nrm(ks[17], (L, A_WIDTH, D_MODEL), A_WIDTH ** -0.5),
        "w_ob": nrm(ks[18], (L, B_WIDTH, D_MODEL), B_WIDTH ** -0.5),
        "w_out": nrm(ks[19], (L, D_MODEL, D_MODEL), D_MODEL ** -0.5),
        "norm_mlp": gain(ks[20], (L, D_MODEL)),
        "router_w": nrm(ks[21], (L, D_MODEL, N_EXPERTS), D_MODEL ** -0.5),
        "router_b": nrm(ks[22], (L, N_EXPERTS), 0.01),
        "w_gate": nrm(ks[23], (L, N_EXPERTS, D_MODEL, D_EXPERT), D_MODEL ** -0.5),
        "b_gate": nrm(ks[24], (L, N_EXPERTS, D_EXPERT), 0.02),
        "w_up": nrm(ks[25], (L, N_EXPERTS, D_MODEL, D_EXPERT), D_MODEL ** -0.5),
        "b_up": nrm(ks[26], (L, N_EXPERTS, D_EXPERT), 0.02),
        "w_down": nrm(ks[27], (L, N_EXPERTS, D_EXPERT, D_MODEL), D_EXPERT ** -0.5),
        "b_down": nrm(ks[28], (L, N_EXPERTS, D_MODEL), 0.02),
    }


def reference(x, c, ctx, c_ctx, w_ada, b_ada, norm_attn, w_in, q_norm_a, k_norm_a, q_norm_b,
              k_norm_b, lambda_q1, lambda_k1, lambda_q2, lambda_k2, subln_b, w_oa, w_ob, w_out,
              norm_mlp, router_w, router_b, w_gate, b_gate, w_up, b_up, w_down, b_down):
    b, s, d = x.shape
    rows = s // GRID_W
    cos, sin = _axial_rope(rows)
    cx = ctx
    for l in range(DEPTH):
        last = l == DEPTH - 1
        lam_init = 0.8 - 0.6 * math.exp(-0.3 * l)
        lam = (jnp.exp(jnp.sum(lambda_q1[l].astype(jnp.float32) * lambda_k1[l].astype(jnp.float32)))
               - jnp.exp(jnp.sum(lambda_q2[l].astype(jnp.float32) * lambda_k2[l].astype(jnp.float32)))
               + lam_init)
        mod_x = (jax.nn.silu(c) @ w_ada[l] + b_ada[l])[:, None, :]
        mod_c = (jax.nn.silu(c_ctx) @ w_ada[l] + b_ada[l])[None, None, :]
        sh_a, sc_a, g_a, sh_m, sc_m, g_m = jnp.split(mod_x, N_MOD, axis=-1)
        csh_a, csc_a, cg_a, csh_m, csc_m, cg_m = jnp.split(mod_c, N_MOD, axis=-1)
        proj = functools.partial(_project, w_in=w_in[l], q_norm_a=q_norm_a[l], k_norm_a=k_norm_a[l],
                                 q_norm_b=q_norm_b[l], k_norm_b=k_norm_b[l])

        hx = _modulate(_rmsnorm(x, norm_attn[l]), sh_a, sc_a)
        hc = _modulate(_rmsnorm(cx, norm_attn[l]), csh_a, csc_a)
        qa, ka, va, qb, kb, vb, ga, gb = proj(hx)
        cqa, cka, cva, cqb, ckb, cvb, cga, cgb = proj(hc)
        qa, ka = _apply_rope(qa, cos, sin), _apply_rope(ka, cos, sin)
        qb, kb = _apply_rope(qb, cos, sin), _apply_rope(kb, cos, sin)
        ka_all = jnp.concatenate([ka, cka], axis=1)
        va_all = jnp.concatenate([va, cva], axis=1)
        kb_all = jnp.concatenate([kb, ckb], axis=1)
        vb_all = jnp.concatenate([vb, cvb], axis=1)
        ya = _sweep_query_blocks(_gqa_attend, qa, ka_all, va_all)
        yb = _sweep_query_blocks(lambda qq, kk, vv: _diff_attend(qq, kk, vv, lam), qb, kb_all, vb_all)
        yb = _diff_out(yb, subln_b[l], lam_init)
        x = x + g_a * _merge(ya, yb, ga, gb, w_oa[l], w_ob[l], w_out[l])
        if not last:
            cya = _gqa_attend(cqa, cka, cva)
            cyb = _diff_out(_diff_attend(cqb, ckb, cvb, lam), subln_b[l], lam_init)
            cx = cx + cg_a * _merge(cya, cyb, cga, cgb, w_oa[l], w_ob[l], w_out[l])

        moe = functools.partial(_moe, router_w=router_w[l], router_b=router_b[l], w_gate=w_gate[l],
                                b_gate=b_gate[l], w_up=w_up[l], b_up=b_up[l], w_down=w_down[l],
                                b_down=b_down[l])
        hx = _modulate(_rmsnorm(x, norm_mlp[l]), sh_m, sc_m)
        x = x + g_m * moe(hx.reshape(b * s, d)).reshape(b, s, d)
        if not last:
            hc = _modulate(_rmsnorm(cx, norm_mlp[l]), csh_m, csc_m)
            cx = cx + cg_m * moe(hc.reshape(-1, d)).reshape(cx.shape)
    return x
```

```python
import contextlib
import math
import numpy as np
import concourse.bass as bass
import concourse.mybir as mybir
from concourse.bass_utils import run_bass_kernel_spmd

F32 = mybir.dt.float32
BF16 = mybir.dt.bfloat16
I32 = mybir.dt.int32
AF = mybir.ActivationFunctionType
ALU = mybir.AluOpType
AX = mybir.AxisListType

NCORES = 8
D = 1024
SEQ = 8192
OWN = 2048
NOWN = OWN // 128
CTX = 256
NKT = (SEQ + CTX) // 128
NKEY = SEQ + CTX
NE = 32
CAP = 512
NT = CAP // 128
NSLOT = NE * CAP
EPS = 1e-6
SUBLN_EPS = 1e-5
LAM_INIT = 0.2
DEBUG = False
STOP_AFTER = 99
HW_ = D


class Buf:
    __slots__ = ("name", "w", "rs")

    def __init__(self, name):
        self.name = name
        self.w = None
        self.rs = []


class Prog:
    ENGS = ("pe", "act", "dve", "pool", "sp")

    def __init__(self, nc, stack):
        self.nc = nc
        self.stack = stack
        self.q = {e: [] for e in self.ENGS}
        self.cnt = {e: 0 for e in self.ENGS}
        self.waited = {e: {} for e in self.ENGS}
        self.dma_cnt = {}
        self.sems = {}

    def sem(self, key):
        s = self.sems.get(key)
        if s is None:
            s = self.stack.enter_context(self.nc.semaphore(f"s{len(self.sems)}"))
            self.sems[key] = s
        return s

    def record(self, f):
        self.rec = []
        f()
        r, self.rec = self.rec, None
        return r

    def replay(self, traces, width=2):
        traces = list(traces)
        n = len(traces)
        done = [False] * n
        posted = [False] * n
        active = []
        nxt = 0

        def flush_posts():
            for i in range(n):
                if not done[i]:
                    break
                if not posted[i]:
                    posted[i] = True
                    for o in traces[i][1]:
                        self.op(*o)
        while nxt < n or active:
            while len(active) < width and nxt < n:
                active.append((nxt, iter(traces[nxt][0]))); nxt += 1
            for ent in list(active):
                i, it = ent
                o = next(it, None)
                if o is None:
                    active.remove(ent)
                    done[i] = True
                    flush_posts()
                else:
                    self.op(*o)
        flush_posts()

    def op(self, eng, fn, reads=(), writes=(), dma=None):
        if getattr(self, "rec", None) is not None:
            self.rec.append((eng, fn, tuple(reads), tuple(writes), dma))
            return None
        deps = {}
        for b in reads:
            if b.w is not None:
                k, v = b.w
                if deps.get(k, 0) < v:
                    deps[k] = v
        for b in writes:
            if b.w is not None:
                k, v = b.w
                if deps.get(k, 0) < v:
                    deps[k] = v
            for (k, v) in b.rs:
                if deps.get(k, 0) < v:
                    deps[k] = v
        if dma is None:
            self.cnt[eng] += 1
            ev = (("E", eng), self.cnt[eng])
            inc = 1
        else:
            self.dma_cnt[dma] = self.dma_cnt.get(dma, 0) + 16
            ev = (("D", dma), self.dma_cnt[dma])
            inc = 16
        waits = []
        wd = self.waited[eng]
        for k, v in deps.items():
            if eng == "pe" and k == ("E", "pe"):
                continue
            if wd.get(k, 0) >= v:
                continue
            wd[k] = v
            waits.append((self.sem(k), v))
        self.q[eng].append((waits, fn, self.sem(ev[0]), inc))
        for b in reads:
            b.rs.append(ev)
        for b in writes:
            b.w = ev
            b.rs = []
        return ev

    def dma_group(self, key, items):
        bufs = []
        for eng, fn, ws in items:
            self.op(eng, fn, dma=key)
            bufs += list(ws)
        for b in bufs:
            b.w = (("D", key), self.dma_cnt[key])
            b.rs = []

    def barrier(self):
        allv = [(("E", e), self.cnt[e]) for e in self.ENGS if self.cnt[e] > 0]
        allv += [(("D", k), v) for k, v in self.dma_cnt.items()]
        for e in self.ENGS:
            waits = []
            wd = self.waited[e]
            for k, v in allv:
                if k == ("E", e):
                    continue
                if wd.get(k, 0) >= v:
                    continue
                wd[k] = v
                waits.append((self.sem(k), v))
            if waits:
                self.q[e].append((waits, None, None, 0))

    def emit(self):
        nc = self.nc
        q = self.q
        with nc.Block() as block:
            def run(eng_obj, lst):
                for waits, fn, sem, inc in lst:
                    for (s, v) in waits:
                        eng_obj.wait_ge(s, v)
                    if fn is not None:
                        fn(eng_obj).then_inc(sem, inc)

            @block.tensor
            def _(e):
                run(e, q["pe"])

            @block.scalar
            def _(e):
                run(e, q["act"])

            @block.vector
            def _(e):
                run(e, q["dve"])

            @block.gpsimd
            def _(e):
                run(e, q["pool"])

            @block.sync
            def _(e):
                run(e, q["sp"])
        self.q = {e: [] for e in self.ENGS}


class Ring:
    def __init__(self, items):
        self.items = items
        self.i = 0

    def next(self):
        it = self.items[self.i % len(self.items)]
        self.i += 1
        return it


def build_program():
    nc = bass.Bass("TRN2", target_bir_lowering=False)
    okind = "ExternalOutput" if DEBUG else "Internal"

    def din(name, shape, dt=F32):
        return nc.dram_tensor(name, list(shape), dt, kind="ExternalInput").ap()

    xp = din("xp", [SEQ, D])
    ctx_d = din("ctx", [CTX, D])
    rope_d = din("rope", [SEQ, 128])
    cvec_d = din("cvec", [128, 16])
    w_ada = din("w_ada", [D, 6 * D])
    b_ada_l = din("b_ada_l", [128, 48])
    nrm_a_l = din("nrm_a_l", [128, 8])
    nrm_m_l = din("nrm_m_l", [128, 8])
    w_in = din("w_in", [D, 4352])
    qk_gain = din("qk_gain", [4, 64])
    lam_v = din("lam_v", [4, 64])
    subln_d = din("subln", [128])
    w_oa = din("w_oa", [512, D])
    w_ob = din("w_ob", [512, D])
    w_out = din("w_out", [D, D])
    router_w = din("router_w", [D, NE])
    router_b = din("router_b", [NE])
    wshape = [NE, D, D] if STOP_AFTER > 3 else [1, 8, 8]
    w_gate = din("w_gate", wshape)
    w_up = din("w_up", wshape)
    w_down = din("w_down", wshape)
    bg_l = din("bg_l", [128, NE * 8])
    bu_l = din("bu_l", [128, NE * 8])
    b_down = din("b_down", [NE, D])
    out_d = nc.dram_tensor("out", [OWN, D], F32, kind="ExternalOutput").ap()

    KTs = nc.dram_tensor("KTs", [5, 128, NKEY], BF16, kind=okind).ap()
    Vs = nc.dram_tensor("Vs", [5, 128, NKT * 130], BF16, kind=okind).ap()
    X1s = nc.dram_tensor("X1s", [OWN, D], F32, kind=okind).ap()
    H2s = nc.dram_tensor("H2s", [OWN, D], BF16, kind=okind).ap()
    Ydbg = nc.dram_tensor("Ydbg", [OWN, D], BF16, kind=okind).ap()
    if DEBUG:
        dbg_hT = nc.dram_tensor("dbg_hT", [128, 1024], BF16, kind="ExternalOutput").ap()
        dbg_xs = nc.dram_tensor("dbg_xs", [128, 1024], BF16, kind="ExternalOutput").ap()
        dbg_mod = nc.dram_tensor("dbg_mod", [128, 96], F32, kind="ExternalOutput").ap()
        dbg_vec = nc.dram_tensor("dbg_vec", [128, 16], F32, kind="ExternalOutput").ap()
        dbg_QT = nc.dram_tensor("dbg_QT", [128, 8 * OWN], BF16, kind="ExternalOutput").ap()
    b_KTs, b_Vs, b_X1s, b_H2s = (Buf(n) for n in ("KTs", "Vs", "X1s", "H2s"))

    top = contextlib.ExitStack()
    with top:
        P = Prog(nc, top)

        def sb(st, name, shape, dt):
            return st.enter_context(nc.sbuf_tensor("s_" + name, list(shape), dt)).ap()

        def mkring(st, name, n, shape, dt):
            return Ring([(sb(st, f"{name}{i}", shape, dt), Buf(f"{name}{i}")) for i in range(n)])

        DB = [top.enter_context(nc.psum_tensor(f"db{i}", [128, 1024], F32)).ap() for i in range(4)]
        DBb = [(Buf(f"db{i}a"), Buf(f"db{i}b")) for i in range(4)]

        def bank(i):
            return DB[i // 2][:, (i % 2) * 512:(i % 2 + 1) * 512], DBb[i // 2][i % 2]

        ident_bf = sb(top, "ident_bf", [128, 128], BF16); b_identbf = Buf("identbf")
        ident32 = sb(top, "ident32", [128, 128], F32); b_ident32 = Buf("ident32")
        ones32 = sb(top, "ones32", [128, 128], F32); b_ones32 = Buf("ones32")
        ones_bf = sb(top, "ones_bf", [128, 128], BF16); b_onesbf = Buf("onesbf")
        gains = sb(top, "gains", [128, 4, 64], F32); b_gains = Buf("gains")
        gsub = sb(top, "gsub", [128, 128], F32); b_gsub = Buf("gsub")
        rb_b = sb(top, "rb_b", [128, NE], F32); b_rbb = Buf("rbb")
        nlam = sb(top, "nlam", [128, 1], F32); b_nlam = Buf("nlam")
        mod_sb = sb(top, "mod_sb", [128, 48, 2], F32); b_mod = Buf("mod")
        Ga = sb(top, "Ga", [128, 8], F32); Gca = sb(top, "Gca", [128, 8], F32)
        SHa = sb(top, "SHa", [128, 8], F32); SHca = sb(top, "SHca", [128, 8], F32)
        Gm = sb(top, "Gm", [128, 8], F32); SHm = sb(top, "SHm", [128, 8], F32)
        gAv = sb(top, "gAv", [128, 8], F32); gMv = sb(top, "gMv", [128, 8], F32)
        b_vecs = Buf("vecs")
        b_bc = Buf("bcast")
        QT = sb(top, "QT", [128, 8, OWN], BF16); b_QT = Buf("QT")
        QTf = QT.rearrange("p c t -> p (c t)").bitcast(F32)
        gA_b, gM_b, GmB, SHmB = (QTf[:, i * D:(i + 1) * D] for i in range(4))
        yA = sb(top, "yA", [128, NOWN, 512], BF16); yB = sb(top, "yB", [128, NOWN, 512], BF16)
        b_y = [Buf(f"y{t}") for t in range(NOWN)]
        WdR = sb(top, "WdR", [128, NOWN, NE], F32); b_WdR = [Buf(f"wdr{t}") for t in range(NOWN)]

        ph0 = contextlib.ExitStack()
        with ph0:

            for t_, b_ in ((ident_bf, b_identbf), (ident32, b_ident32)):
                P.op("pool", lambda e, t_=t_: e.memset(t_, 1.0), writes=[b_])
                P.op("pool", lambda e, t_=t_: e.affine_select(out=t_, in_=t_, pattern=[[-1, 128]], compare_op=ALU.is_equal,
                                                              fill=0.0, base=0, channel_multiplier=1), reads=[b_], writes=[b_])
            P.op("dve", lambda e: e.memset(ones32, 1.0), writes=[b_ones32])
            P.op("dve", lambda e: e.memset(ones_bf, 1.0), writes=[b_onesbf])
            cgrp = [("sp", lambda e: e.dma_start(out=rb_b, in_=router_b.partition_broadcast(128)), [b_rbb])]
            for i in range(4):
                cgrp.append(("sp", lambda e, i=i: e.dma_start(out=gains[:, i, :], in_=qk_gain[i].partition_broadcast(128)), [b_gains]))
            lamt = sb(ph0, "lamt", [128, 4, 64], F32); b_lamt = Buf("lamt")
            lamp = sb(ph0, "lamp", [128, 2, 64], F32); b_lamp = Buf("lamp")
            lams = sb(ph0, "lams", [128, 2], F32); b_lams = Buf("lams")
            lame = sb(ph0, "lame", [128, 2], F32); b_lame = Buf("lame")
            subt = sb(ph0, "subt", [128, 128], F32); b_subt = Buf("subt")
            for i in range(4):
                cgrp.append(("sp", lambda e, i=i: e.dma_start(out=lamt[:, i, :], in_=lam_v[i].partition_broadcast(128)), [b_lamt]))
            cgrp.append(("sp", lambda e: e.dma_start(out=subt, in_=subln_d.partition_broadcast(128)), [b_subt]))
            cv = sb(ph0, "cv", [128, 16], F32); b_cv = Buf("cv")
            cvs = sb(ph0, "cvs", [128, 16], F32); b_cvs = Buf("cvs")
            bal = sb(ph0, "bal", [128, 48], F32); b_bal = Buf("bal")
            nal = sb(ph0, "nal", [128, 8], F32); nml = sb(ph0, "nml", [128, 8], F32); b_nl = Buf("nl")
            cgrp.append(("sp", lambda e: e.dma_start(out=cv, in_=cvec_d), [b_cv]))
            cgrp.append(("sp", lambda e: e.dma_start(out=bal, in_=b_ada_l), [b_bal]))
            cgrp.append(("sp", lambda e: e.dma_start(out=nal, in_=nrm_a_l), [b_nl]))
            cgrp.append(("sp", lambda e: e.dma_start(out=nml, in_=nrm_m_l), [b_nl]))
            P.dma_group("c", cgrp)
            P.op("dve", lambda e: e.tensor_scalar(out=gsub, in0=subt, scalar1=1.0 - LAM_INIT, scalar2=None, op0=ALU.mult),
                 reads=[b_subt], writes=[b_gsub])
            lam4 = lamt.rearrange("p (a b) d -> p a b d", b=2)
            P.op("dve", lambda e: e.tensor_tensor(out=lamp, in0=lam4[:, :, 0, :], in1=lam4[:, :, 1, :], op=ALU.mult),
                 reads=[b_lamt], writes=[b_lamp])
            P.op("dve", lambda e: e.tensor_reduce(out=lams, in_=lamp, axis=AX.X, op=ALU.add), reads=[b_lamp], writes=[b_lams])
            P.op("act", lambda e: e.activation(out=lame, in_=lams, func=AF.Exp), reads=[b_lams], writes=[b_lame])
            P.op("dve", lambda e: e.tensor_tensor(out=nlam, in0=lame[:, 1:2], in1=lame[:, 0:1], op=ALU.subtract),
                 reads=[b_lame], writes=[b_nlam])
            P.op("dve", lambda e: e.tensor_scalar(out=nlam, in0=nlam, scalar1=-LAM_INIT, scalar2=None, op0=ALU.add),
                 reads=[b_nlam], writes=[b_nlam])

            P.op("act", lambda e: e.activation(out=cvs, in_=cv, func=AF.Exp, scale=-1.0), reads=[b_cv], writes=[b_cvs])
            P.op("dve", lambda e: e.tensor_scalar(out=cvs, in0=cvs, scalar1=1.0, scalar2=None, op0=ALU.add), reads=[b_cvs], writes=[b_cvs])
            P.op("dve", lambda e: e.reciprocal(out=cvs, in_=cvs), reads=[b_cvs], writes=[b_cvs])
            P.op("dve", lambda e: e.tensor_tensor(out=cvs, in0=cvs, in1=cv, op=ALU.mult), reads=[b_cvs, b_cv], writes=[b_cvs])
            wk_ring = mkring(ph0, "wadak", 2, [128, 8, D], F32)
            modps, b_modps = bank(0)
            modv = modps[:, 0:96].rearrange("p (m w) -> p m w", w=2)
            w_ada_v = w_ada.rearrange("(k p) n -> p k n", p=128)
            for blk in range(6):
                wk, b_wk = wk_ring.next()
                P.op("sp", lambda e, wk=wk, blk=blk: e.dma_start(out=wk, in_=w_ada_v[:, :, blk * D:(blk + 1) * D]),
                     writes=[b_wk], dma=f"wada{blk % 2}")

                def mm(e, wk=wk, blk=blk):
                    ins = None
                    for mm_ in range(8):
                        m = blk * 8 + mm_
                        for k in range(8):
                            ins = e.matmul(modv[:, m, :], lhsT=wk[:, k, mm_ * 128:(mm_ + 1) * 128], rhs=cvs[:, 2 * k:2 * k + 2],
                                           start=(k == 0), stop=(k == 7))
                    return ins
                P.op("pe", mm, reads=[b_wk, b_cvs], writes=[b_modps])
            P.op("dve", lambda e: e.tensor_tensor(out=mod_sb, in0=modv, in1=bal.unsqueeze(2).to_broadcast([128, 48, 2]), op=ALU.add),
                 reads=[b_modps, b_bal], writes=[b_mod])

            def vecs(e):
                e.scalar_tensor_tensor(out=Ga, in0=mod_sb[:, 8:16, 0], scalar=1.0, in1=nal, op0=ALU.add, op1=ALU.mult)
                e.scalar_tensor_tensor(out=Gca, in0=mod_sb[:, 8:16, 1], scalar=1.0, in1=nal, op0=ALU.add, op1=ALU.mult)
                e.scalar_tensor_tensor(out=Gm, in0=mod_sb[:, 32:40, 0], scalar=1.0, in1=nml, op0=ALU.add, op1=ALU.mult)
                e.tensor_copy(out=SHa, in_=mod_sb[:, 0:8, 0])
                e.tensor_copy(out=SHca, in_=mod_sb[:, 0:8, 1])
                e.tensor_copy(out=SHm, in_=mod_sb[:, 24:32, 0])
                e.tensor_copy(out=gAv, in_=mod_sb[:, 16:24, 0])
                return e.tensor_copy(out=gMv, in_=mod_sb[:, 40:48, 0])
            P.op("dve", vecs, reads=[b_mod, b_nl], writes=[b_vecs])
            P.barrier()
            P.emit()

        def norm_to_hT(xt, b_xt, G, SH, bufs):
            junk, b_junk, ss, b_ss, xs, b_xs, hT, b_hT, tp, b_tp = bufs
            P.op("act", lambda e: e.activation(out=junk, in_=xt, func=AF.Square, accum_out=ss), reads=[b_xt], writes=[b_junk, b_ss])
            P.op("act", lambda e: e.activation(out=ss, in_=ss, func=AF.Ln, scale=1.0 / D, bias=EPS), reads=[b_ss], writes=[b_ss])
            P.op("act", lambda e: e.activation(out=ss, in_=ss, func=AF.Exp, scale=-0.5), reads=[b_ss], writes=[b_ss])
            P.op("dve", lambda e: e.tensor_scalar(out=xs, in0=xt, scalar1=ss, scalar2=None, op0=ALU.mult), reads=[b_xt, b_ss], writes=[b_xs])
            tpb = tp.bitcast(BF16).rearrange("p (k t) -> p k t", t=128)

            def tr(e):
                ins = None
                for k in range(8):
                    ins = e.transpose(out=tpb[:, k, :], in_=xs[:, k * 128:(k + 1) * 128], identity=ident_bf)
                return ins
            P.op("pe", tr, reads=[b_xs, b_identbf], writes=[b_tp])

            def ev(e):
                ins = None
                for k in range(8):
                    ins = e.activation(out=hT[:, k, :], in_=tpb[:, k, :], func=AF.Identity, scale=G[:, k:k + 1], bias=SH[:, k:k + 1])
                return ins
            P.op("act", ev, reads=[b_tp, b_vecs], writes=[b_hT])

        ph1 = contextlib.ExitStack()
        with ph1:
            Wkvq = sb(ph1, "Wkvq", [128, 8, 2304], BF16); b_Wkvq = Buf("Wkvq")
            P.dma_group("wkvq", [("pool", (lambda e, k=k: e.dma_start(out=Wkvq[:, k, :], in_=w_in[k * 128:(k + 1) * 128, 0:2304])), [b_Wkvq])
                                 for k in range(8)])
            def lanes(name, n, shape, dt):
                return [mkring(ph1, f"{name}L{l}_", n, shape, dt) for l in range(2)]
            XT = lanes("xt", 2, [128, D], F32)
            RT = lanes("rt", 2, [128, 128], F32)
            JK = mkring(ph1, "junk", 1, [128, D], BF16)
            SS = lanes("ss", 1, [128, 1], F32)
            XS = lanes("xs", 1, [128, D], BF16)
            HT = lanes("hT", 1, [128, 8, 128], BF16)
            SQ = lanes("sq", 2, [128, 512], F32)
            S8 = lanes("s8", 2, [128, 8], F32)
            XG = lanes("xg", 2, [128, 512], F32)
            RA = lanes("ra", 2, [128, 512], F32)
            RB = lanes("rbm", 2, [128, 512], F32)
            KTM = lanes("ktm", 1, [128, 640], BF16)
            QTM = lanes("qtm", 1, [128, 1024], BF16)
            KG = mkring(ph1, "kg", 2, [128, 5, 512], BF16)
            VG = mkring(ph1, "vg", 2, [128, 5, 4 * 130], BF16)
            for (vg, b_vg) in VG.items:
                P.op("pool", lambda e, vg=vg: e.memset(vg, 1.0), writes=[b_vg])
            TPB = [Ring([bank(0)]), Ring([bank(1)])]
            PJ = [Ring([bank(2), bank(3)]), Ring([bank(4), bank(5)])]
            KTP = [Ring([bank(6)]), Ring([bank(7)])]

            def normrope(ln, ps, b_ps, H, gi, rt, b_rt, out4, b_out, perm=None):
                W = H * 64
                sq, b_sq = SQ[ln].next(); s8, b_s8 = S8[ln].next(); xg, b_xg = XG[ln].next()
                v3 = lambda a: a[:, 0:W].rearrange("p (h d) -> p h d", d=64)
                P.op("act", lambda e: e.activation(out=sq[:, 0:W], in_=ps, func=AF.Square), reads=[b_ps], writes=[b_sq])
                P.op("dve", lambda e: e.tensor_reduce(out=s8[:, 0:H], in_=v3(sq), axis=AX.X, op=ALU.add), reads=[b_sq], writes=[b_s8])
                P.op("act", lambda e: e.activation(out=s8[:, 0:H], in_=s8[:, 0:H], func=AF.Ln, scale=1.0 / 64, bias=EPS), reads=[b_s8], writes=[b_s8])
                P.op("act", lambda e: e.activation(out=s8[:, 0:H], in_=s8[:, 0:H], func=AF.Exp, scale=-0.5), reads=[b_s8], writes=[b_s8])
                g = gains[:, gi, :]
                P.op("dve", lambda e: e.tensor_tensor(out=v3(xg), in0=ps.rearrange("p (h d) -> p h d", d=64),
                                                      in1=g.unsqueeze(1).to_broadcast([128, H, 64]), op=ALU.mult),
                     reads=[b_ps, b_gains], writes=[b_xg])
                src, b_src = xg, b_xg
                if rt is not None:
                    ra, b_ra = RA[ln].next(); rbm, b_rbm = RB[ln].next()
                    cosF = rt[:, 0:64]
                    sinS = rt[:, 64:128].rearrange("p (i two) -> p i two", two=2)
                    P.op("dve", lambda e: e.tensor_tensor(out=v3(ra), in0=v3(xg), in1=cosF.unsqueeze(1).to_broadcast([128, H, 64]), op=ALU.mult),
                         reads=[b_xg, b_rt], writes=[b_ra])
                    x4 = xg[:, 0:W].rearrange("p (h i two) -> p h i two", i=32, two=2)
                    r4 = rbm[:, 0:W].rearrange("p (h i two) -> p h i two", i=32, two=2)

                    def sw(e):
                        e.tensor_tensor(out=r4[:, :, :, 0], in0=x4[:, :, :, 1], in1=sinS[:, :, 0].unsqueeze(1).to_broadcast([128, H, 32]), op=ALU.mult)
                        return e.tensor_tensor(out=r4[:, :, :, 1], in0=x4[:, :, :, 0], in1=sinS[:, :, 1].unsqueeze(1).to_broadcast([128, H, 32]), op=ALU.mult)
                    P.op("dve", sw, reads=[b_xg, b_rt], writes=[b_rbm])
                    P.op("dve", lambda e: e.tensor_tensor(out=ra[:, 0:W], in0=ra[:, 0:W], in1=rbm[:, 0:W], op=ALU.add),
                         reads=[b_ra, b_rbm], writes=[b_ra])
                    src, b_src = ra, b_ra
                if perm is None:
                    P.op("dve", lambda e: e.tensor_tensor(out=out4, in0=v3(src), in1=s8[:, 0:H].unsqueeze(2).to_broadcast([128, H, 64]), op=ALU.mult),
                         reads=[b_src, b_s8], writes=[b_out])
                else:
                    s4 = src[:, 0:W].rearrange("p (g r d) -> p g r d", g=2, d=64)
                    r4b = s8[:, 0:H].rearrange("p (g r) -> p g r", g=2).unsqueeze(3).to_broadcast([128, 2, 4, 64])
                    P.op("dve", lambda e: e.tensor_tensor(out=out4, in0=s4, in1=r4b, op=ALU.mult), reads=[b_src, b_s8], writes=[b_out])

            def proj(ln, hT, b_hT, c0, c1):
                ps, b_ps = PJ[ln].next()
                W = c1 - c0

                def mm(e):
                    ins = None
                    for k in range(8):
                        ins = e.matmul(ps[:, 0:W], lhsT=hT[:, k, :], rhs=Wkvq[:, k, c0:c1], start=(k == 0), stop=(k == 7))
                    return ins
                P.op("pe", mm, reads=[b_hT, b_Wkvq], writes=[b_ps])
                return ps, b_ps

            def front(ti):
                is_ctx = ti >= 64
                ln = ti % 2
                xt, b_xt = XT[ln].next()
                src = ctx_d[(ti - 64) * 128:(ti - 63) * 128, :] if is_ctx else xp[ti * 128:(ti + 1) * 128, :]
                P.op("sp", lambda e, xt=xt, src=src: e.dma_start(out=xt, in_=src), writes=[b_xt], dma=f"xt{ln}_{(XT[ln].i - 1) % 2}")
                rt, b_rt = (None, None)
                if not is_ctx:
                    rt, b_rt = RT[ln].next()
                    P.op("sp", lambda e, rt=rt, ti=ti: e.dma_start(out=rt, in_=rope_d[ti * 128:(ti + 1) * 128, :]),
                         writes=[b_rt], dma=f"rt{ln}_{(RT[ln].i - 1) % 2}")
                junk, b_junk = JK.next(); ss, b_ss = SS[ln].next(); xs, b_xs = XS[ln].next(); hT, b_hT = HT[ln].next(); tp, b_tp = TPB[ln].next()
                norm_to_hT(xt, b_xt, Gca if is_ctx else Ga, SHca if is_ctx else SHa,
                           (junk, b_junk, ss, b_ss, xs, b_xs, hT, b_hT, tp, b_tp))
                if DEBUG and ti == 0:
                    P.op("sp", lambda e, hT=hT: e.dma_start(out=dbg_hT, in_=hT.rearrange("p k t -> p (k t)")), reads=[b_hT], dma="dbg1")
                    P.op("sp", lambda e, xs=xs: e.dma_start(out=dbg_xs, in_=xs), reads=[b_xs], dma="dbg1")
                    P.op("sp", lambda e: e.dma_start(out=dbg_mod, in_=mod_sb.rearrange("p m w -> p (m w)")), reads=[b_mod], dma="dbg1")
                    P.op("sp", lambda e: e.dma_start(out=dbg_vec[:, 0:8], in_=Ga), reads=[b_vecs], dma="dbg1")
                    P.op("sp", lambda e: e.dma_start(out=dbg_vec[:, 8:16], in_=SHa), reads=[b_vecs], dma="dbg1")
                return hT, b_hT, rt, b_rt

            ngrp = (NKT + 3) // 4
            traces1 = []
            for grp in range(ngrp):
                t0 = grp * 4
                nt = min(4, NKT - t0)
                kg, b_kg = KG.next(); vg, b_vg = VG.next()
                vg4 = vg.rearrange("p s (t c) -> p s t c", c=130)
                for tg in range(nt):
                  def tile_body(grp=grp, t0=t0, nt=nt, kg=kg, b_kg=b_kg, vg=vg, b_vg=b_vg, vg4=vg4, tg=tg):
                    ti = t0 + tg
                    is_ctx = ti >= 64
                    own = ti < NOWN
                    hT, b_hT, rt, b_rt = front(ti)
                    ln = ti % 2
                    ktm, b_ktm = KTM[ln].next()
                    ps, b_ps = proj(ln, hT, b_hT, 512, 768)
                    normrope(ln, ps[:, 0:128], b_ps, 2, 1, rt, b_rt, ktm[:, 0:128].rearrange("p (h d) -> p h d", d=64), b_ktm)
                    vA = vg4[:, 0, tg, :].rearrange("p (m d) -> p m d", d=65)[:, :, 0:64]
                    P.op("act", lambda e, vA=vA, ps=ps: e.activation(out=vA, in_=ps[:, 128:256].rearrange("p (m d) -> p m d", d=64), func=AF.Copy),
                         reads=[b_ps], writes=[b_vg])
                    ps, b_ps = proj(ln, hT, b_hT, 1280, 1792)
                    normrope(ln, ps, b_ps, 8, 3, rt, b_rt, ktm[:, 128:640].rearrange("p (h d) -> p h d", d=64), b_ktm)
                    ps, b_ps = proj(ln, hT, b_hT, 1792, 2304)
                    vB4 = vg4[:, 1:5, tg, 0:128]
                    P.op("act", lambda e, vB4=vB4, ps=ps: e.activation(out=vB4, in_=ps.rearrange("p (s d) -> p s d", d=128), func=AF.Copy),
                         reads=[b_ps], writes=[b_vg])
                    kp, b_kp = KTP[ln].next()
                    kpb = kp.bitcast(BF16).rearrange("p (c t) -> p c t", t=128)

                    def ktr(e, ktm=ktm, kpb=kpb):
                        ins = None
                        for c in range(5):
                            ins = e.transpose(out=kpb[:, c, :], in_=ktm[:, c * 128:(c + 1) * 128], identity=ident_bf)
                        return ins
                    P.op("pe", ktr, reads=[b_ktm, b_identbf], writes=[b_kp])
                    P.op("dve", lambda e, kg=kg, kpb=kpb, tg=tg: e.tensor_copy(out=kg[:, :, tg * 128:(tg + 1) * 128], in_=kpb[:, 0:5, :]),
                         reads=[b_kp], writes=[b_kg])
                    if own:
                        qtm, b_qtm = QTM[ln].next()
                        ps, b_ps = proj(ln, hT, b_hT, 0, 512)
                        normrope(ln, ps, b_ps, 8, 0, rt, b_rt, qtm[:, 0:512].rearrange("p (r g d) -> p g r d", g=2, d=64), b_qtm, perm=True)
                        ps, b_ps = proj(ln, hT, b_hT, 768, 1280)
                        normrope(ln, ps, b_ps, 8, 2, rt, b_rt, qtm[:, 512:1024].rearrange("p (h d) -> p h d", d=64), b_qtm)
                        kp, b_kp = KTP[ln].next()
                        qpb = kp.bitcast(BF16).rearrange("p (c t) -> p c t", t=128)

                        def qtr(e, qtm=qtm, qpb=qpb):
                            ins = None
                            for c in range(8):
                                ins = e.transpose(out=qpb[:, c, :], in_=qtm[:, c * 128:(c + 1) * 128], identity=ident_bf)
                            return ins
                        P.op("pe", qtr, reads=[b_qtm, b_identbf], writes=[b_kp])
                        P.op("act", lambda e, qpb=qpb, ti=ti: e.activation(out=QT[:, :, ti * 128:(ti + 1) * 128], in_=qpb, func=AF.Copy),
                             reads=[b_kp], writes=[b_QT])
                  def stores(grp=grp, t0=t0, nt=nt, kg=kg, b_kg=b_kg, vg=vg, b_vg=b_vg):
                    P.op("sp", lambda e: e.dma_start(out=KTs[:, :, t0 * 128:(t0 + nt) * 128].rearrange("c p t -> p c t"), in_=kg[:, :, 0:nt * 128]),
                         reads=[b_kg], writes=[b_KTs], dma=f"kst{grp % 2}")
                    P.op("sp", lambda e: e.dma_start(out=Vs[:, :, t0 * 130:(t0 + nt) * 130].rearrange("c p t -> p c t"), in_=vg[:, :, 0:nt * 130]),
                         reads=[b_vg], writes=[b_Vs], dma=f"vst{grp % 2}")
                  traces1.append((P.record(tile_body), P.record(stores) if tg == nt - 1 else []))
            P.replay(traces1, width=2)
            if DEBUG:
                P.op("sp", lambda e: e.dma_start(out=dbg_QT, in_=QT.rearrange("p c t -> p (c t)")), reads=[b_QT], dma="dbg1")
            P.barrier()
            P.emit()

        if STOP_AFTER <= 1:
            return nc
        ph2 = contextlib.ExitStack()
        with ph2:
            KV = [(sb(ph2, f"KT{i}", [128, NKEY], BF16), sb(ph2, f"V{i}", [128, NKT, 130], BF16), Buf(f"kv{i}")) for i in range(2)]
            PB = mkring(ph2, "pb", 6, [128, 1024], BF16)
            OS = mkring(ph2, "osb", 1, [65, 1024], F32)
            RD = mkring(ph2, "rden", 2, [128, 2], F32)
            NL = mkring(ph2, "nl", 2, [128, 1], F32)
            O1 = mkring(ph2, "o1", 2, [128, 128], F32)
            OO = mkring(ph2, "oo", 2, [128, 128], F32)
            JS = mkring(ph2, "js", 1, [128, 128], F32)
            SSB = mkring(ph2, "ssb", 2, [128, 1], F32)
            PP2 = mkring(ph2, "pp2", 2, [128, 1024], BF16)
            ACCD = [(sb(ph2, f"accd{i}", [128, 1024], F32), Buf(f"accd{i}")) for i in range(2)]
            OSB = mkring(ph2, "osbB", 1, [128, 1024], F32)
            SR = Ring([(DB[0], DBb[0]), (DB[1], DBb[1])])

            def load_kv(L):
                KTt, Vt, b_kv = KV[L % 2]
                P.op("sp", lambda e: e.dma_start(out=KTt, in_=KTs[L]), reads=[b_KTs], writes=[b_kv], dma=f"kvk{L % 2}")
                P.op("sp", lambda e: e.dma_start(out=Vt.rearrange("p t c -> p (t c)"), in_=Vs[L]), reads=[b_Vs], writes=[b_kv], dma=f"kvv{L % 2}")

            load_kv(0)
            load_kv(1)
            fin_i = 0
            blk_i = 0
            pending = []

            def drain(n):
                for _ in range(n):
                    if pending:
                        P.op(*pending.pop(0))
            for u in range(8):
                isB = u >= 4
                L = 0 if not isB else 1 + (u - 4)
                KTt, Vt, b_kv = KV[L % 2]
                for qb in range(4):
                    q0 = qb * 512
                    accd, b_accd = ACCD[blk_i % 2]; blk_i += 1

                    def qk(kt):
                        S, (bs0, bs1) = SR.next()

                        def f(e, S=S, kt=kt, KTt=KTt, u=u, q0=q0):
                            e.matmul(S[:, 0:512], lhsT=KTt[0:64, kt * 128:(kt + 1) * 128], rhs=QT[0:64, u, q0:q0 + 512], start=True, stop=True)
                            return e.matmul(S[:, 512:1024], lhsT=KTt[64:128, kt * 128:(kt + 1) * 128], rhs=QT[64:128, u, q0:q0 + 512], start=True, stop=True)
                        P.op("pe", f, reads=[b_kv, b_QT], writes=[bs0, bs1])
                        return S, bs0, bs1

                    sq_ = [qk(0), qk(1)]
                    for kt in range(NKT):
                        if kt >= 2:
                            drain(2)
                        S, bs0, bs1 = sq_.pop(0)
                        pb, b_pb = PB.next()
                        P.op("act", lambda e, pb=pb, S=S: e.activation(out=pb, in_=S, func=AF.Exp, scale=0.125), reads=[bs0, bs1], writes=[b_pb])
                        if kt + 2 < NKT:
                            sq_.append(qk(kt + 2))
                        st, sp_ = (kt == 0), (kt == NKT - 1)
                        if not isB:
                            def pv(e, pb=pb, kt=kt, st=st, sp_=sp_, Vt=Vt):
                                e.matmul(DB[2][0:65, 0:512], lhsT=Vt[:, kt, 0:65], rhs=pb[:, 0:512], start=st, stop=sp_)
                                return e.matmul(DB[2][0:65, 512:1024], lhsT=Vt[:, kt, 65:130], rhs=pb[:, 512:1024], start=st, stop=sp_)
                            P.op("pe", pv, reads=[b_pb, b_kv], writes=[DBb[2][0], DBb[2][1]])
                        else:
                            def pv(e, pb=pb, kt=kt, st=st, sp_=sp_, Vt=Vt):
                                e.matmul(DB[2][:, 0:512], lhsT=Vt[:, kt, 0:128], rhs=pb[:, 0:512], start=st, stop=sp_)
                                return e.matmul(DB[2][:, 512:1024], lhsT=Vt[:, kt, 0:128], rhs=pb[:, 512:1024], start=st, stop=sp_)
                            P.op("pe", pv, reads=[b_pb, b_kv], writes=[DBb[2][0], DBb[2][1]])
                            if kt % 2 == 0:
                                pb_prev, b_pb_prev = pb, b_pb
                            else:
                                pp2, b_pp2 = PP2.next()
                                P.op("dve", lambda e, pp2=pp2, pb=pb, pb_prev=pb_prev: e.tensor_tensor(out=pp2, in0=pb, in1=pb_prev, op=ALU.add),
                                     reads=[b_pb, b_pb_prev], writes=[b_pp2])
                                if kt == 1:
                                    P.op("dve", lambda e, pp2=pp2, accd=accd: e.tensor_copy(out=accd, in_=pp2), reads=[b_pp2], writes=[b_accd])
                                else:
                                    P.op("dve", lambda e, pp2=pp2, accd=accd: e.tensor_tensor(out=accd, in0=accd, in1=pp2, op=ALU.add), reads=[b_pp2, b_accd], writes=[b_accd])
                    drain(len(pending))
                    if not isB:
                        os2, b_os2 = OS.next()
                        P.op("dve", lambda e, os2=os2: e.tensor_copy(out=os2, in_=DB[2][0:65, :]), reads=[DBb[2][0], DBb[2][1]], writes=[b_os2])
                    else:
                        osB, b_osB = OSB.next()
                        P.op("dve", lambda e, osB=osB: e.tensor_copy(out=osB, in_=DB[2]), reads=[DBb[2][0], DBb[2][1]], writes=[b_osB])
                    P.rec = []
                    for qi in range(4):
                        qt = qb * 4 + qi
                        fb, b_fb = bank(6 + fin_i % 2); fin_i += 1
                        c0 = qi * 128
                        if not isB:
                            F3 = fb[:, 0:260].rearrange("p (m c) -> p m c", c=130)

                            def ftr(e, os2=os2, F3=F3, c0=c0):
                                e.transpose(out=F3[:, 0, 0:65], in_=os2[0:65, c0:c0 + 128], identity=ident32[0:65, 0:65])
                                return e.transpose(out=F3[:, 1, 0:65], in_=os2[0:65, 512 + c0:512 + c0 + 128], identity=ident32[0:65, 0:65])
                            P.op("pe", ftr, reads=[b_os2, b_ident32], writes=[b_fb])
                            rd, b_rd = RD.next()
                            P.op("dve", lambda e, rd=rd, F3=F3: e.reciprocal(out=rd, in_=F3[:, :, 64]), reads=[b_fb], writes=[b_rd])
                            yo = yA[:, qt, :].rearrange("p (m r d) -> p m r d", m=2, d=64)[:, :, u, :]
                            P.op("dve", lambda e, rd=rd, F3=F3, yo=yo: e.tensor_tensor(out=yo, in0=F3[:, :, 0:64], in1=rd.unsqueeze(2).to_broadcast([128, 2, 64]), op=ALU.mult),
                                 reads=[b_fb, b_rd], writes=[b_y[qt]])
                        else:
                            def ftr(e, osB=osB, fb=fb, c0=c0, accd=accd):
                                e.transpose(out=fb[:, 0:128], in_=osB[:, c0:c0 + 128], identity=ident32)
                                e.transpose(out=fb[:, 128:256], in_=osB[:, 512 + c0:512 + c0 + 128], identity=ident32)
                                e.matmul(fb[:, 256:257], lhsT=accd[:, c0:c0 + 128], rhs=ones32[:, 0:1], start=True, stop=True)
                                return e.matmul(fb[:, 257:258], lhsT=accd[:, 512 + c0:512 + c0 + 128], rhs=ones32[:, 0:1], start=True, stop=True)
                            P.op("pe", ftr, reads=[b_osB, b_ident32, b_accd, b_ones32], writes=[b_fb])
                            rd, b_rd = RD.next(); nl, b_nl = NL.next(); o1, b_o1 = O1.next(); oo, b_oo = OO.next()
                            js, b_js = JS.next(); ssb, b_ssb = SSB.next()
                            P.op("dve", lambda e, rd=rd, fb=fb: e.reciprocal(out=rd, in_=fb[:, 256:258]), reads=[b_fb], writes=[b_rd])
                            P.op("dve", lambda e, rd=rd, nl=nl: e.tensor_tensor(out=nl, in0=rd[:, 1:2], in1=nlam, op=ALU.mult), reads=[b_rd, b_nlam], writes=[b_nl])
                            P.op("dve", lambda e, o1=o1, fb=fb, rd=rd: e.tensor_scalar(out=o1, in0=fb[:, 0:128], scalar1=rd[:, 0:1], scalar2=None, op0=ALU.mult),
                                 reads=[b_fb, b_rd], writes=[b_o1])
                            P.op("dve", lambda e, oo=oo, o1=o1, fb=fb, nl=nl: e.scalar_tensor_tensor(
                                out=oo, in0=fb[:, 128:256], scalar=nl, in1=o1, op0=ALU.mult, op1=ALU.add),
                                reads=[b_fb, b_nl, b_o1], writes=[b_oo])
                            P.op("act", lambda e, js=js, oo=oo, ssb=ssb: e.activation(out=js, in_=oo, func=AF.Square, accum_out=ssb), reads=[b_oo], writes=[b_js, b_ssb])
                            P.op("act", lambda e, ssb=ssb: e.activation(out=ssb, in_=ssb, func=AF.Ln, scale=1.0 / 128, bias=SUBLN_EPS), reads=[b_ssb], writes=[b_ssb])
                            P.op("act", lambda e, ssb=ssb: e.activation(out=ssb, in_=ssb, func=AF.Exp, scale=-0.5), reads=[b_ssb], writes=[b_ssb])
                            hb = u - 4
                            P.op("dve", lambda e, oo=oo, ssb=ssb, qt=qt, hb=hb: e.scalar_tensor_tensor(
                                out=yB[:, qt, hb * 128:(hb + 1) * 128], in0=oo, scalar=ssb, in1=gsub, op0=ALU.mult, op1=ALU.mult),
                                reads=[b_oo, b_ssb, b_gsub], writes=[b_y[qt]])
                    pending.extend(P.rec); P.rec = None
                if u == 3:
                    load_kv(2)
                elif 4 <= u < 6:
                    load_kv(u - 4 + 3)
            drain(len(pending))
            if DEBUG:
                for t in range(NOWN):
                    P.op("sp", lambda e, t=t: e.dma_start(out=Ydbg[t * 128:(t + 1) * 128, 0:512], in_=yA[:, t, :]), reads=[b_y[t]], dma="dbgy")
                    P.op("sp", lambda e, t=t: e.dma_start(out=Ydbg[t * 128:(t + 1) * 128, 512:1024], in_=yB[:, t, :]), reads=[b_y[t]], dma="dbgy")
            P.barrier()
            P.emit()

        if STOP_AFTER <= 2:
            return nc
        ph3 = contextlib.ExitStack()
        with ph3:
            Wg_ = sb(ph3, "Wgate", [128, 8, 2048], BF16); b_Wg_ = Buf("Wgate")
            Woa = sb(ph3, "Woa", [128, 4, D], BF16); Wob = sb(ph3, "Wob", [128, 4, D], BF16); b_Wo = Buf("Wo")
            Wout = sb(ph3, "Wout", [128, 8, D], BF16); b_Wout = Buf("Wout")
            rw32 = sb(ph3, "rw32", [128, 8, NE], F32); b_rw = Buf("rw")
            P.dma_group("w3a", [("pool", (lambda e, k=k: e.dma_start(out=Wg_[:, k, :], in_=w_in[k * 128:(k + 1) * 128, 2304:4352])), [b_Wg_])
                                for k in range(8)])
            w3 = []
            for k in range(4):
                w3.append(("pool", (lambda e, k=k: e.dma_start(out=Woa[:, k, :], in_=w_oa[k * 128:(k + 1) * 128, :])), [b_Wo]))
                w3.append(("pool", (lambda e, k=k: e.dma_start(out=Wob[:, k, :], in_=w_ob[k * 128:(k + 1) * 128, :])), [b_Wo]))
            for k in range(8):
                w3.append(("pool", (lambda e, k=k: e.dma_start(out=Wout[:, k, :], in_=w_out[k * 128:(k + 1) * 128, :])), [b_Wout]))
            P.dma_group("w3", w3)
            P.op("sp", lambda e: e.dma_start(out=rw32, in_=router_w.rearrange("(k p) n -> p k n", p=128)), writes=[b_rw], dma="rw")
            dg_ring = mkring(ph3, "dg", 2, [128, 128], F32)
            bi = 1
            for (vec, dst) in ((gAv, gA_b), (gMv, gM_b), (Gm, GmB), (SHm, SHmB)):
                for k in range(8):
                    dg, b_dg = dg_ring.next()
                    pb, b_pb = bank(1 + (bi % 2)); bi += 1
                    P.op("dve", lambda e, dg=dg, vec=vec, k=k: e.tensor_scalar(out=dg, in0=ident32, scalar1=vec[:, k:k + 1], scalar2=None, op0=ALU.mult),
                         reads=[b_ident32, b_vecs], writes=[b_dg])
                    P.op("pe", lambda e, dg=dg, pb=pb: e.matmul(pb[:, 0:128], lhsT=ones32, rhs=dg, start=True, stop=True),
                         reads=[b_ones32, b_dg], writes=[b_pb])
                    P.op("act", lambda e, dst=dst, pb=pb, k=k: e.activation(out=dst[:, k * 128:(k + 1) * 128], in_=pb[:, 0:128], func=AF.Copy),
                         reads=[b_pb], writes=[b_bc])
            XT = mkring(ph3, "xt3", 2, [128, D], F32)
            JK = mkring(ph3, "junk3", 1, [128, D], BF16)
            SS = mkring(ph3, "ss3", 4, [128, 1], F32)
            XS = mkring(ph3, "xs3", 2, [128, D], BF16)
            HT = mkring(ph3, "hT3", 2, [128, 8, 128], BF16)
            SG = mkring(ph3, "sg", 2, [128, 2048], F32)
            YT = mkring(ph3, "yT", 1, [128, 8, 128], BF16)
            M1 = mkring(ph3, "m1", 1, [128, D], F32)
            MG = mkring(ph3, "mg", 1, [128, D], BF16)
            MT = mkring(ph3, "mT", 1, [128, 8, 128], BF16)
            X1 = mkring(ph3, "x1", 2, [128, D], F32)
            XS2 = mkring(ph3, "xs2", 1, [128, D], F32)
            H2B = mkring(ph3, "h2b", 2, [128, HW_], BF16)
            H2T = mkring(ph3, "h2T", 1, [128, 8, 128], F32)
            LG = mkring(ph3, "lg", 2, [128, NE], F32)
            M8 = mkring(ph3, "m8", 2, [128, 8], F32)
            MK = mkring(ph3, "mk", 2, [128, NE], F32)
            EX = mkring(ph3, "ex", 2, [128, NE], F32)
            SM = mkring(ph3, "sm", 2, [128, 2], F32)

            def stage1(t):
                xt, b_xt = XT.next()
                P.op("sp", lambda e, xt=xt, t=t: e.dma_start(out=xt, in_=xp[t * 128:(t + 1) * 128, :]), writes=[b_xt], dma=f"xt3{t % 2}")
                junk, b_junk = JK.next(); ss, b_ss = SS.next(); xs, b_xs = XS.next(); hT, b_hT = HT.next()
                tp, b_tp = bank(0)
                norm_to_hT(xt, b_xt, Ga, SHa, (junk, b_junk, ss, b_ss, xs, b_xs, hT, b_hT, tp, b_tp))
                sg, b_sg = SG.next()
                for gb in range(4):
                    ps, b_ps = bank(2 + gb % 2)

                    def mm(e, ps=ps, gb=gb, hT=hT):
                        ins = None
                        for k in range(8):
                            ins = e.matmul(ps, lhsT=hT[:, k, :], rhs=Wg_[:, k, gb * 512:(gb + 1) * 512], start=(k == 0), stop=(k == 7))
                        return ins
                    P.op("pe", mm, reads=[b_hT, b_Wg_], writes=[b_ps])
                    sgs = sg[:, gb * 512:(gb + 1) * 512]
                    P.op("act", lambda e, sgs=sgs, ps=ps: e.activation(out=sgs, in_=ps, func=AF.Sigmoid), reads=[b_ps], writes=[b_sg])
                return xt, b_xt, sg, b_sg

            st1 = {0: stage1(0)}
            x1s = {}

            def s2(t, xt, b_xt, sg, b_sg):
                yT, b_yT = YT.next()
                tp, b_tp = bank(1)
                tpb = tp.bitcast(BF16).rearrange("p (k t) -> p k t", t=128)

                def ytr(e, tpb=tpb, t=t):
                    ins = None
                    for k in range(4):
                        e.transpose(out=tpb[:, k, :], in_=yA[:, t, k * 128:(k + 1) * 128], identity=ident_bf)
                        ins = e.transpose(out=tpb[:, 4 + k, :], in_=yB[:, t, k * 128:(k + 1) * 128], identity=ident_bf)
                    return ins
                P.op("pe", ytr, reads=[b_y[t], b_identbf], writes=[b_tp])
                P.op("dve", lambda e, yT=yT, tpb=tpb: e.tensor_copy(out=yT, in_=tpb), reads=[b_tp], writes=[b_yT])
                m1, b_m1 = M1.next(); mg, b_mg = MG.next()
                for cb in range(2):
                    pa, b_pa = bank(4 + cb); pbk, b_pbk = bank(6 + cb)

                    def mmo(e, pa=pa, pbk=pbk, cb=cb, yT=yT):
                        for k in range(4):
                            e.matmul(pa, lhsT=yT[:, k, :], rhs=Woa[:, k, cb * 512:(cb + 1) * 512], start=(k == 0), stop=(k == 3))
                        ins = None
                        for k in range(4):
                            ins = e.matmul(pbk, lhsT=yT[:, 4 + k, :], rhs=Wob[:, k, cb * 512:(cb + 1) * 512], start=(k == 0), stop=(k == 3))
                        return ins
                    P.op("pe", mmo, reads=[b_yT, b_Wo], writes=[b_pa, b_pbk])
                    cs = slice(cb * 512, (cb + 1) * 512)
                    P.op("dve", lambda e, m1=m1, pa=pa, sg=sg, cs=cs: e.tensor_tensor(out=m1[:, cs], in0=pa, in1=sg[:, cs], op=ALU.mult),
                         reads=[b_pa, b_sg], writes=[b_m1])
                    P.op("dve", lambda e, sg=sg, pbk=pbk, cb=cb: e.tensor_tensor(out=sg[:, 1024 + cb * 512:1024 + (cb + 1) * 512], in0=pbk, in1=sg[:, 1024 + cb * 512:1024 + (cb + 1) * 512], op=ALU.mult),
                         reads=[b_pbk, b_sg], writes=[b_sg])
                    P.op("dve", lambda e, mg=mg, m1=m1, sg=sg, cs=cs, cb=cb: e.tensor_tensor(out=mg[:, cs], in0=m1[:, cs], in1=sg[:, 1024 + cb * 512:1024 + (cb + 1) * 512], op=ALU.add),
                         reads=[b_m1, b_sg], writes=[b_mg])
                mT, b_mT = MT.next()
                tp, b_tp = bank(1)
                tpb = tp.bitcast(BF16).rearrange("p (k t) -> p k t", t=128)

                def mtr(e, tpb=tpb, mg=mg):
                    ins = None
                    for k in range(8):
                        ins = e.transpose(out=tpb[:, k, :], in_=mg[:, k * 128:(k + 1) * 128], identity=ident_bf)
                    return ins
                P.op("pe", mtr, reads=[b_mg, b_identbf], writes=[b_tp])
                P.op("act", lambda e, mT=mT, tpb=tpb: e.activation(out=mT, in_=tpb, func=AF.Copy), reads=[b_tp], writes=[b_mT])
                x1, b_x1 = X1.next()
                for cb in range(2):
                    ps, b_ps = bank(2 + cb)

                    def mmw(e, ps=ps, cb=cb, mT=mT):
                        ins = None
                        for k in range(8):
                            ins = e.matmul(ps, lhsT=mT[:, k, :], rhs=Wout[:, k, cb * 512:(cb + 1) * 512], start=(k == 0), stop=(k == 7))
                        return ins
                    P.op("pe", mmw, reads=[b_mT, b_Wout], writes=[b_ps])
                    cs = slice(cb * 512, (cb + 1) * 512)
                    P.op("dve", lambda e, x1=x1, ps=ps, cs=cs: e.tensor_tensor(out=x1[:, cs], in0=ps, in1=gA_b[:, cs], op=ALU.mult), reads=[b_ps, b_bc], writes=[b_x1])
                P.op("dve", lambda e, x1=x1, xt=xt: e.tensor_tensor(out=x1, in0=x1, in1=xt, op=ALU.add), reads=[b_x1, b_xt], writes=[b_x1])
                P.op("sp", lambda e, x1=x1, t=t: e.dma_start(out=X1s[t * 128:(t + 1) * 128, :], in_=x1), reads=[b_x1], writes=[b_X1s], dma=f"x1st{t % 2}")
                x1s[t] = (x1, b_x1)

            def s3(t):
                x1, b_x1 = x1s.pop(t)
                ss, b_ss = SS.next(); xs2, b_xs2 = XS2.next(); h2b, b_h2b = H2B.next(); h2T, b_h2T = H2T.next()
                junk, b_junk = JK.next()
                P.op("act", lambda e, junk=junk, x1=x1, ss=ss: e.activation(out=junk, in_=x1, func=AF.Square, accum_out=ss), reads=[b_x1], writes=[b_junk, b_ss])
                P.op("act", lambda e, ss=ss: e.activation(out=ss, in_=ss, func=AF.Ln, scale=1.0 / D, bias=EPS), reads=[b_ss], writes=[b_ss])
                P.op("act", lambda e, ss=ss: e.activation(out=ss, in_=ss, func=AF.Exp, scale=-0.5), reads=[b_ss], writes=[b_ss])
                P.op("dve", lambda e, xs2=xs2, x1=x1, ss=ss: e.scalar_tensor_tensor(out=xs2, in0=x1, scalar=ss, in1=GmB, op0=ALU.mult, op1=ALU.mult),
                     reads=[b_x1, b_ss, b_bc], writes=[b_xs2])
                P.op("dve", lambda e, xs2=xs2: e.tensor_tensor(out=xs2, in0=xs2, in1=SHmB, op=ALU.add), reads=[b_xs2, b_bc], writes=[b_xs2])
                P.op("act", lambda e, h2b=h2b, xs2=xs2: e.activation(out=h2b, in_=xs2, func=AF.Copy), reads=[b_xs2], writes=[b_h2b])
                for half in range(2):
                    tp, b_tp = bank(0)
                    tpv = tp.rearrange("p (k t) -> p k t", t=128)

                    def htr(e, tpv=tpv, xs2=xs2, half=half):
                        ins = None
                        for k in range(4):
                            kk = half * 4 + k
                            ins = e.transpose(out=tpv[:, k, :], in_=xs2[:, kk * 128:(kk + 1) * 128], identity=ident32)
                        return ins
                    P.op("pe", htr, reads=[b_xs2, b_ident32], writes=[b_tp])
                    P.op("act" if half == 0 else "dve",
                         (lambda e, h2T=h2T, tpv=tpv, half=half: e.activation(out=h2T[:, half * 4:(half + 1) * 4, :], in_=tpv, func=AF.Copy)) if half == 0 else
                         (lambda e, h2T=h2T, tpv=tpv, half=half: e.tensor_copy(out=h2T[:, half * 4:(half + 1) * 4, :], in_=tpv)),
                         reads=[b_tp], writes=[b_h2T])
                lp, b_lp = bank(0)
                lg, b_lg = LG.next(); m8, b_m8 = M8.next(); mk, b_mk = MK.next()
                ex, b_ex = EX.next(); sm, b_sm = SM.next()

                def mml(e, lp=lp, h2T=h2T):
                    ins = None
                    for k in range(8):
                        ins = e.matmul(lp[:, 0:NE], lhsT=h2T[:, k, :], rhs=rw32[:, k, :], start=(k == 0), stop=(k == 7))
                    return ins
                P.op("pe", mml, reads=[b_h2T, b_rw], writes=[b_lp])
                P.op("dve", lambda e, lg=lg, lp=lp: e.tensor_tensor(out=lg, in0=lp[:, 0:NE], in1=rb_b, op=ALU.add), reads=[b_lp, b_rbb], writes=[b_lg])
                P.op("dve", lambda e, m8=m8, lg=lg: e.max(out=m8, in_=lg), reads=[b_lg], writes=[b_m8])
                P.op("dve", lambda e, mk=mk, lg=lg, m8=m8: e.tensor_scalar(out=mk, in0=lg, scalar1=m8[:, 3:4], scalar2=None, op0=ALU.is_ge), reads=[b_lg, b_m8], writes=[b_mk])
                P.op("dve", lambda e, sm=sm, m8=m8: e.tensor_scalar(out=sm[:, 0:1], in0=m8[:, 0:1], scalar1=-1.0, scalar2=None, op0=ALU.mult), reads=[b_m8], writes=[b_sm])
                P.op("act", lambda e, ex=ex, lg=lg, sm=sm: e.activation(out=ex, in_=lg, func=AF.Exp, bias=sm[:, 0:1], scale=1.0), reads=[b_lg, b_sm], writes=[b_ex])
                P.op("dve", lambda e, ex=ex, mk=mk: e.tensor_tensor(out=ex, in0=ex, in1=mk, op=ALU.mult), reads=[b_ex, b_mk], writes=[b_ex])
                P.op("dve", lambda e, sm=sm, ex=ex: e.tensor_reduce(out=sm[:, 1:2], in_=ex, axis=AX.X, op=ALU.add), reads=[b_ex], writes=[b_sm])
                P.op("dve", lambda e, sm=sm: e.reciprocal(out=sm[:, 1:2], in_=sm[:, 1:2]), reads=[b_sm], writes=[b_sm])
                P.op("dve", lambda e, ex=ex, sm=sm, t=t: e.tensor_scalar(out=WdR[:, t, :], in0=ex, scalar1=sm[:, 1:2], scalar2=None, op0=ALU.mult), reads=[b_ex, b_sm], writes=[b_WdR[t]])
                P.op("sp", lambda e, h2b=h2b, t=t: e.dma_start(out=H2s[t * 128:(t + 1) * 128, :], in_=h2b), reads=[b_h2b], writes=[b_H2s], dma=f"h2st{t % 2}")

            prev3 = None
            for t in range(NOWN):
                if t + 1 < NOWN:
                    st1[t + 1] = stage1(t + 1)
                xt, b_xt, sg, b_sg = st1.pop(t)
                tr2 = P.record(lambda: s2(t, xt, b_xt, sg, b_sg))
                P.replay(([(prev3, [])] if prev3 is not None else []) + [(tr2, [])], width=2)
                prev3 = P.record(lambda: s3(t))
            P.replay([(prev3, [])], width=1)
            P.barrier()
            P.emit()

        if STOP_AFTER <= 3:
            return nc
        ph4 = contextlib.ExitStack()
        with ph4:
            WS = [(sb(ph4, f"ws{i}", [128, 8, D], BF16), (Buf(f"ws{i}a"), Buf(f"ws{i}b"))) for i in range(4)]
            bg = sb(ph4, "bg", [128, NE, 8], F32); bu1 = sb(ph4, "bu1", [128, NE, 8], F32); b_bgu = Buf("bgu")
            P.dma_group("b4", [("sp", lambda e: e.dma_start(out=bg.rearrange("p e k -> p (e k)"), in_=bg_l), [b_bgu]),
                               ("sp", lambda e: e.dma_start(out=bu1.rearrange("p e k -> p (e k)"), in_=bu_l), [b_bgu])])
            P.op("dve", lambda e: e.tensor_scalar(out=bu1, in0=bu1, scalar1=1.0, scalar2=None, op0=ALU.add), reads=[b_bgu], writes=[b_bgu])
            bdn = sb(ph4, "bdn", [NE, D], F32); b_bdn = Buf("bdn")
            P.op("sp", lambda e: e.dma_start(out=bdn, in_=b_down), writes=[b_bdn], dma="bdn")
            HR = sb(ph4, "hr", [128, 8, D], BF16); b_HR = Buf("hr")
            XTb = sb(ph4, "xtb", [128, 8, 1024], BF16); b_XTb = Buf("xtb")
            ACTT = mkring(ph4, "actT", 2, [128, 8, 512], BF16)
            G1 = mkring(ph4, "g1", 3, [128, 512], F32)
            TS = mkring(ph4, "ts", 2, [128, 512], F32)
            U0 = mkring(ph4, "u0", 2, [128, 512], F32)
            X1 = mkring(ph4, "x1c", 1, [128, D], F32)
            WT = mkring(ph4, "wT", 1, [NE, 128], F32)
            accA = yA.rearrange("p a b -> p (a b)").bitcast(F32).rearrange("p (i d) -> p i d", d=D)
            accB = yB.rearrange("p a b -> p (a b)").bitcast(F32).rearrange("p (i d) -> p i d", d=D)
            acc = [(accA[:, i, :] if i < 4 else accB[:, i - 4, :], Buf(f"acc{i}")) for i in range(8)]
            mats = (w_gate, w_up, w_down)
            NMAT = 2 * NE * 3

            def load_w(j):
                e_, m_ = (j // 3) % NE, j % 3
                w, b_w = WS[j % 4]
                src = mats[m_][e_].rearrange("(k p) n -> p k n", p=128)
                for h in range(2):
                    P.op("pool", lambda e, w=w, src=src, h=h: e.dma_start(out=w[:, h * 4:(h + 1) * 4, :], in_=src[:, h * 4:(h + 1) * 4, :]),
                         writes=[b_w[h]], dma=f"ws{j % 4}_{h}")
            for j in range(4):
                load_w(j)
            nload = 4
            PG = Ring([bank(0), bank(1)])
            PU = Ring([bank(2), bank(3)])
            PY = Ring([bank(4), bank(5)])
            PT = Ring([bank(6), bank(7)])
            for blk in range(2):
                P.op("sp", lambda e, blk=blk: e.dma_start(out=HR, in_=H2s[blk * 1024:(blk + 1) * 1024, :].rearrange("(n p) d -> p n d", p=128)),
                     reads=[b_H2s], writes=[b_HR], dma="hr")
                for n in range(8):
                    tp, b_tp = PT.next()
                    tpb = tp.bitcast(BF16).rearrange("p (k t) -> p k t", t=128)

                    def xtr(e, tpb=tpb, n=n):
                        ins = None
                        for k in range(8):
                            ins = e.transpose(out=tpb[:, k, :], in_=HR[:, n, k * 128:(k + 1) * 128], identity=ident_bf)
                        return ins
                    P.op("pe", xtr, reads=[b_HR, b_identbf], writes=[b_tp])
                    P.op("dve" if n % 2 == 0 else "act",
                         (lambda e, tpb=tpb, n=n: e.tensor_copy(out=XTb[:, :, n * 128:(n + 1) * 128], in_=tpb)) if n % 2 == 0 else
                         (lambda e, tpb=tpb, n=n: e.activation(out=XTb[:, :, n * 128:(n + 1) * 128], in_=tpb, func=AF.Copy)),
                         reads=[b_tp], writes=[b_XTb])
                for ex_ in range(NE):
                    jg = (blk * NE + ex_) * 3
                    Wg, b_Wg = WS[jg % 4]; Wu, b_Wu = WS[(jg + 1) % 4]; Wdn, b_Wdn = WS[(jg + 2) % 4]
                    for half in range(2):
                        xT = XTb[:, :, half * 512:(half + 1) * 512]
                        actT, b_actT = ACTT.next()
                        for j in range(8):
                            pg, b_pg = PG.next(); pu, b_pu = PU.next()

                            def mg_(e, pg=pg, j=j, Wg=Wg, xT=xT):
                                ins = None
                                for k in range(8):
                                    ins = e.matmul(pg, lhsT=Wg[:, k, j * 128:(j + 1) * 128], rhs=xT[:, k, :], start=(k == 0), stop=(k == 7))
                                return ins
                            P.op("pe", mg_, reads=[*b_Wg, b_XTb], writes=[b_pg])

                            def mu_(e, pu=pu, j=j, Wu=Wu, xT=xT):
                                ins = None
                                for k in range(8):
                                    ins = e.matmul(pu, lhsT=Wu[:, k, j * 128:(j + 1) * 128], rhs=xT[:, k, :], start=(k == 0), stop=(k == 7))
                                return ins
                            P.op("pe", mu_, reads=[*b_Wu, b_XTb], writes=[b_pu])
                            g1, b_g1 = G1.next(); ts, b_ts = TS.next(); u0, b_u0 = U0.next()
                            P.op("dve", lambda e, g1=g1, pg=pg, ex_=ex_, j=j: e.tensor_scalar(out=g1, in0=pg, scalar1=bg[:, ex_, j:j + 1], scalar2=7.0, op0=ALU.add, op1=ALU.min),
                                 reads=[b_pg, b_bgu], writes=[b_g1])
                            P.op("act", lambda e, ts=ts, g1=g1: e.activation(out=ts, in_=g1, func=AF.Silu, scale=1.702), reads=[b_g1], writes=[b_ts])
                            P.op("act", lambda e, u0=u0, pu=pu, ex_=ex_, j=j: e.activation(out=u0, in_=pu, func=AF.Identity, bias=bu1[:, ex_, j:j + 1], scale=1.0),
                                 reads=[b_pu, b_bgu], writes=[b_u0])
                            P.op("dve", lambda e, u0=u0: e.tensor_scalar(out=u0, in0=u0, scalar1=-6.0, scalar2=8.0, op0=ALU.max, op1=ALU.min), reads=[b_u0], writes=[b_u0])
                            P.op("dve", lambda e, actT=actT, u0=u0, ts=ts, j=j: e.scalar_tensor_tensor(out=actT[:, j, :], in0=u0, scalar=1.0 / 1.702, in1=ts, op0=ALU.mult, op1=ALU.mult),
                                 reads=[b_u0, b_ts], writes=[b_actT])
                        if half == 1:
                            while nload < min(NMAT, jg + 6):
                                load_w(nload); nload += 1
                        for n in range(4):
                            i = half * 4 + n
                            t = blk * 8 + i
                            a_ap, b_a = acc[i]
                            wsc = WdR[:, t, ex_:ex_ + 1]
                            for cb in range(2):
                                py, b_py = PY.next()
                                cs = slice(cb * 512, (cb + 1) * 512)

                                def md_(e, py=py, n=n, cb=cb, Wdn=Wdn, actT=actT):
                                    ins = None
                                    for j in range(8):
                                        ins = e.matmul(py, lhsT=actT[:, j, n * 128:(n + 1) * 128], rhs=Wdn[:, j, cb * 512:(cb + 1) * 512], start=(j == 0), stop=(j == 7))
                                    return ins
                                P.op("pe", md_, reads=[*b_Wdn, b_actT], writes=[b_py])
                                if ex_ == 0:
                                    P.op("dve", lambda e, a_ap=a_ap, py=py, wsc=wsc, cs=cs: e.tensor_scalar(out=a_ap[:, cs], in0=py, scalar1=wsc, scalar2=None, op0=ALU.mult),
                                         reads=[b_py, b_WdR[t]], writes=[b_a])
                                else:
                                    P.op("dve", lambda e, a_ap=a_ap, py=py, wsc=wsc, cs=cs: e.scalar_tensor_tensor(out=a_ap[:, cs], in0=py, scalar=wsc, in1=a_ap[:, cs], op0=ALU.mult, op1=ALU.add),
                                         reads=[b_py, b_WdR[t], b_a], writes=[b_a])
                    while nload < min(NMAT, jg + 7):
                        load_w(nload); nload += 1
                for i in range(8):
                    t = blk * 8 + i
                    a_ap, b_a = acc[i]
                    x1, b_x1 = X1.next()
                    P.op("sp", lambda e, x1=x1, t=t: e.dma_start(out=x1, in_=X1s[t * 128:(t + 1) * 128, :]), reads=[b_X1s], writes=[b_x1], dma="x1c")
                    wT, b_wT = WT.next()
                    tp, b_tp = PT.next()
                    P.op("pe", lambda e, tp=tp, t=t: e.transpose(out=tp[0:NE, 0:128], in_=WdR[:, t, :], identity=ident32), reads=[b_WdR[t], b_ident32], writes=[b_tp])
                    P.op("act", lambda e, wT=wT, tp=tp: e.activation(out=wT, in_=tp[0:NE, 0:128], func=AF.Copy), reads=[b_tp], writes=[b_wT])
                    for cb in range(2):
                        pbd, b_pbd = PY.next()
                        cs = slice(cb * 512, (cb + 1) * 512)
                        P.op("pe", lambda e, pbd=pbd, wT=wT, cs=cs: e.matmul(pbd, lhsT=wT, rhs=bdn[:, cs], start=True, stop=True), reads=[b_wT, b_bdn], writes=[b_pbd])
                        P.op("dve", lambda e, a_ap=a_ap, pbd=pbd, cs=cs: e.tensor_tensor(out=a_ap[:, cs], in0=pbd, in1=a_ap[:, cs], op=ALU.add), reads=[b_pbd, b_a], writes=[b_a])
                    P.op("dve", lambda e, a_ap=a_ap: e.tensor_tensor(out=a_ap, in0=a_ap, in1=gM_b, op=ALU.mult), reads=[b_a, b_bc], writes=[b_a])
                    P.op("dve", lambda e, a_ap=a_ap, x1=x1: e.tensor_tensor(out=a_ap, in0=a_ap, in1=x1, op=ALU.add), reads=[b_a, b_x1], writes=[b_a])
                    P.op("sp", lambda e, a_ap=a_ap, t=t: e.dma_start(out=out_d[t * 128:(t + 1) * 128, :], in_=a_ap), reads=[b_a], dma=f"ost{i}")
            P.barrier()
            P.emit()
    return nc


def _rope_tables():
    rows = SEQ // 64
    row = np.repeat(np.arange(rows, dtype=np.float32), 64)
    col = np.tile(np.arange(64, dtype=np.float32), rows)
    inv = (1.0 / (10000.0 ** (np.arange(16, dtype=np.float32) / 16))).astype(np.float32)
    ang = np.concatenate([row[:, None] * inv, col[:, None] * inv], axis=-1).astype(np.float32)
    cos, sin = np.cos(ang).astype(np.float32), np.sin(ang).astype(np.float32)
    tab = np.zeros((SEQ, 128), np.float32)
    tab[:, 0:64:2] = cos
    tab[:, 1:64:2] = cos
    tab[:, 64:128:2] = -sin
    tab[:, 65:128:2] = sin
    return tab


def _chunk_layout(v):
    return np.ascontiguousarray(v.reshape(-1, 128).T)


def make_in_maps(inputs):
    f = lambda a: np.ascontiguousarray(np.asarray(a, dtype=np.float32))
    x = f(inputs["x"]); c = f(inputs["c"]); ctx = f(inputs["ctx"]); c_ctx = f(inputs["c_ctx"])
    rope = _rope_tables()
    shared = {
        "w_ada": f(inputs["w_ada"][0]),
        "b_ada_l": _chunk_layout(f(inputs["b_ada"][0])),
        "nrm_a_l": _chunk_layout(f(inputs["norm_attn"][0])),
        "nrm_m_l": _chunk_layout(f(inputs["norm_mlp"][0])),
        "w_in": f(inputs["w_in"][0]),
        "qk_gain": np.stack([f(inputs[k][0]) for k in ("q_norm_a", "k_norm_a", "q_norm_b", "k_norm_b")]),
        "lam_v": np.stack([f(inputs[k][0]) for k in ("lambda_q1", "lambda_k1", "lambda_q2", "lambda_k2")]),
        "subln": f(inputs["subln_b"][0]),
        "w_oa": f(inputs["w_oa"][0]), "w_ob": f(inputs["w_ob"][0]), "w_out": f(inputs["w_out"][0]),
        "router_w": f(inputs["router_w"][0]), "router_b": f(inputs["router_b"][0]),
        "w_gate": f(inputs["w_gate"][0]), "w_up": f(inputs["w_up"][0]), "w_down": f(inputs["w_down"][0]),
        "bg_l": np.ascontiguousarray(f(inputs["b_gate"][0]).reshape(NE, 8, 128).transpose(2, 0, 1).reshape(128, NE * 8)),
        "bu_l": np.ascontiguousarray(f(inputs["b_up"][0]).reshape(NE, 8, 128).transpose(2, 0, 1).reshape(128, NE * 8)),
        "b_down": f(inputs["b_down"][0]),
    }
    maps = []
    for i in range(NCORES):
        b, j = i // 4, i % 4
        order = np.concatenate([np.arange(j * OWN, (j + 1) * OWN), np.arange(0, j * OWN), np.arange((j + 1) * OWN, SEQ)])
        cvec = np.empty((128, 16), np.float32)
        cvec[:, 0::2] = _chunk_layout(c[b])
        cvec[:, 1::2] = _chunk_layout(c_ctx)
        m = dict(shared)
        m["xp"] = np.ascontiguousarray(x[b][order])
        m["ctx"] = ctx[b]
        m["rope"] = np.ascontiguousarray(rope[order])
        m["cvec"] = cvec
        maps.append(m)
    return maps


_NC_CACHE = {}


def kernel(**inputs):
    if "nc" not in _NC_CACHE:
        _NC_CACHE["nc"] = build_program()
    nc = _NC_CACHE["nc"]
    in_maps = make_in_maps(inputs)
    res = run_bass_kernel_spmd(nc, in_maps, core_ids=list(range(NCORES)))
    out = np.empty((2, SEQ, D), np.float32)
    for i in range(NCORES):
        b, j = i // 4, i % 4
        out[b, j * OWN:(j + 1) * OWN] = res.results[i]["out"]
    if DEBUG:
        _NC_CACHE["res"] = res
    return out
```

```python
import contextlib
import math
import numpy as np
import concourse.bass as bass
import concourse.mybir as mybir
from concourse.bass_utils import run_bass_kernel_spmd

F32 = mybir.dt.float32
BF16 = mybir.dt.bfloat16
I32 = mybir.dt.int32
AF = mybir.ActivationFunctionType
ALU = mybir.AluOpType
AX = mybir.AxisListType

NCORES = 8
D = 1024
SEQ = 8192
OWN = 2048
NOWN = OWN // 128
CTX = 256
NKT = (SEQ + CTX) // 128
NKEY = SEQ + CTX
NE = 32
CAP = 512
NT = CAP // 128
NSLOT = NE * CAP
EPS = 1e-6
SUBLN_EPS = 1e-5
LAM_INIT = 0.2
DEBUG = False
STOP_AFTER = 99
HW_ = D


class Buf:
    __slots__ = ("name", "w", "rs")

    def __init__(self, name):
        self.name = name
        self.w = None
        self.rs = []


class Prog:
    ENGS = ("pe", "act", "dve", "pool", "sp")

    def __init__(self, nc, stack):
        self.nc = nc
        self.stack = stack
        self.q = {e: [] for e in self.ENGS}
        self.cnt = {e: 0 for e in self.ENGS}
        self.waited = {e: {} for e in self.ENGS}
        self.dma_cnt = {}
        self.sems = {}

    def sem(self, key):
        s = self.sems.get(key)
        if s is None:
            s = self.stack.enter_context(self.nc.semaphore(f"s{len(self.sems)}"))
            self.sems[key] = s
        return s

    def record(self, f):
        self.rec = []
        f()
        r, self.rec = self.rec, None
        return r

    def replay(self, traces, width=2):
        traces = list(traces)
        n = len(traces)
        done = [False] * n
        posted = [False] * n
        active = []
        nxt = 0

        def flush_posts():
            for i in range(n):
                if not done[i]:
                    break
                if not posted[i]:
                    posted[i] = True
                    for o in traces[i][1]:
                        self.op(*o)
        while nxt < n or active:
            while len(active) < width and nxt < n:
                active.append((nxt, iter(traces[nxt][0]))); nxt += 1
            for ent in list(active):
                i, it = ent
                o = next(it, None)
                if o is None:
                    active.remove(ent)
                    done[i] = True
                    flush_posts()
                else:
                    self.op(*o)
        flush_posts()

    def op(self, eng, fn, reads=(), writes=(), dma=None):
        if getattr(self, "rec", None) is not None:
            self.rec.append((eng, fn, tuple(reads), tuple(writes), dma))
            return None
        deps = {}
        for b in reads:
            if b.w is not None:
                k, v = b.w
                if deps.get(k, 0) < v:
                    deps[k] = v
        for b in writes:
            if b.w is not None:
                k, v = b.w
                if deps.get(k, 0) < v:
                    deps[k] = v
            for (k, v) in b.rs:
                if deps.get(k, 0) < v:
                    deps[k] = v
        if dma is None:
            self.cnt[eng] += 1
            ev = (("E", eng), self.cnt[eng])
            inc = 1
        else:
            self.dma_cnt[dma] = self.dma_cnt.get(dma, 0) + 16
            ev = (("D", dma), self.dma_cnt[dma])
            inc = 16
        waits = []
        wd = self.waited[eng]
        for k, v in deps.items():
            if eng == "pe" and k == ("E", "pe"):
                continue
            if wd.get(k, 0) >= v:
                continue
            wd[k] = v
            waits.append((self.sem(k), v))
        self.q[eng].append((waits, fn, self.sem(ev[0]), inc))
        for b in reads:
            b.rs.append(ev)
        for b in writes:
            b.w = ev
            b.rs = []
        return ev

    def dma_group(self, key, items):
        bufs = []
        for eng, fn, ws in items:
            self.op(eng, fn, dma=key)
            bufs += list(ws)
        for b in bufs:
            b.w = (("D", key), self.dma_cnt[key])
            b.rs = []

    def barrier(self):
        allv = [(("E", e), self.cnt[e]) for e in self.ENGS if self.cnt[e] > 0]
        allv += [(("D", k), v) for k, v in self.dma_cnt.items()]
        for e in self.ENGS:
            waits = []
            wd = self.waited[e]
            for k, v in allv:
                if k == ("E", e):
                    continue
                if wd.get(k, 0) >= v:
                    continue
                wd[k] = v
                waits.append((self.sem(k), v))
            if waits:
                self.q[e].append((waits, None, None, 0))

    def emit(self):
        nc = self.nc
        q = self.q
        with nc.Block() as block:
            def run(eng_obj, lst):
                for waits, fn, sem, inc in lst:
                    for (s, v) in waits:
                        eng_obj.wait_ge(s, v)
                    if fn is not None:
                        fn(eng_obj).then_inc(sem, inc)

            @block.tensor
            def _(e):
                run(e, q["pe"])

            @block.scalar
            def _(e):
                run(e, q["act"])

            @block.vector
            def _(e):
                run(e, q["dve"])

            @block.gpsimd
            def _(e):
                run(e, q["pool"])

            @block.sync
            def _(e):
                run(e, q["sp"])
        self.q = {e: [] for e in self.ENGS}


class Ring:
    def __init__(self, items):
        self.items = items
        self.i = 0

    def next(self):
        it = self.items[self.i % len(self.items)]
        self.i += 1
        return it


def build_program():
    nc = bass.Bass("TRN2", target_bir_lowering=False)
    okind = "ExternalOutput" if DEBUG else "Internal"

    def din(name, shape, dt=F32):
        return nc.dram_tensor(name, list(shape), dt, kind="ExternalInput").ap()

    xp = din("xp", [SEQ, D])
    ctx_d = din("ctx", [CTX, D])
    rope_d = din("rope", [SEQ, 128])
    cvec_d = din("cvec", [128, 16])
    w_ada = din("w_ada", [D, 6 * D])
    b_ada_l = din("b_ada_l", [128, 48])
    nrm_a_l = din("nrm_a_l", [128, 8])
    nrm_m_l = din("nrm_m_l", [128, 8])
    w_in = din("w_in", [D, 4352])
    qk_gain = din("qk_gain", [4, 64])
    lam_v = din("lam_v", [4, 64])
    subln_d = din("subln", [128])
    w_oa = din("w_oa", [512, D])
    w_ob = din("w_ob", [512, D])
    w_out = din("w_out", [D, D])
    router_w = din("router_w", [D, NE])
    router_b = din("router_b", [NE])
    wshape = [NE, D, D] if STOP_AFTER > 3 else [1, 8, 8]
    w_gate = din("w_gate", wshape)
    w_up = din("w_up", wshape)
    w_down = din("w_down", wshape)
    bg_l = din("bg_l", [128, NE * 8])
    bu_l = din("bu_l", [128, NE * 8])
    b_down = din("b_down", [NE, D])
    out_d = nc.dram_tensor("out", [OWN, D], F32, kind="ExternalOutput").ap()

    KTs = nc.dram_tensor("KTs", [5, 128, NKEY], BF16, kind=okind).ap()
    Vs = nc.dram_tensor("Vs", [5, 128, NKT * 130], BF16, kind=okind).ap()
    X1s = nc.dram_tensor("X1s", [OWN, D], F32, kind=okind).ap()
    H2s = nc.dram_tensor("H2s", [OWN, D], BF16, kind=okind).ap()
    Ydbg = nc.dram_tensor("Ydbg", [OWN, D], BF16, kind=okind).ap()
    if DEBUG:
        dbg_hT = nc.dram_tensor("dbg_hT", [128, 1024], BF16, kind="ExternalOutput").ap()
        dbg_xs = nc.dram_tensor("dbg_xs", [128, 1024], BF16, kind="ExternalOutput").ap()
        dbg_mod = nc.dram_tensor("dbg_mod", [128, 96], F32, kind="ExternalOutput").ap()
        dbg_vec = nc.dram_tensor("dbg_vec", [128, 16], F32, kind="ExternalOutput").ap()
        dbg_QT = nc.dram_tensor("dbg_QT", [128, 8 * OWN], BF16, kind="ExternalOutput").ap()
    b_KTs, b_Vs, b_X1s, b_H2s = (Buf(n) for n in ("KTs", "Vs", "X1s", "H2s"))

    top = contextlib.ExitStack()
    with top:
        P = Prog(nc, top)

        def sb(st, name, shape, dt):
            return st.enter_context(nc.sbuf_tensor("s_" + name, list(shape), dt)).ap()

        def mkring(st, name, n, shape, dt):
            return Ring([(sb(st, f"{name}{i}", shape, dt), Buf(f"{name}{i}")) for i in range(n)])

        DB = [top.enter_context(nc.psum_tensor(f"db{i}", [128, 1024], F32)).ap() for i in range(4)]
        DBb = [(Buf(f"db{i}a"), Buf(f"db{i}b")) for i in range(4)]

        def bank(i):
            return DB[i // 2][:, (i % 2) * 512:(i % 2 + 1) * 512], DBb[i // 2][i % 2]

        ident_bf = sb(top, "ident_bf", [128, 128], BF16); b_identbf = Buf("identbf")
        ident32 = sb(top, "ident32", [128, 128], F32); b_ident32 = Buf("ident32")
        ones32 = sb(top, "ones32", [128, 128], F32); b_ones32 = Buf("ones32")
        ones_bf = sb(top, "ones_bf", [128, 128], BF16); b_onesbf = Buf("onesbf")
        gains = sb(top, "gains", [128, 4, 64], F32); b_gains = Buf("gains")
        gsub = sb(top, "gsub", [128, 128], F32); b_gsub = Buf("gsub")
        rb_b = sb(top, "rb_b", [128, NE], F32); b_rbb = Buf("rbb")
        nlam = sb(top, "nlam", [128, 1], F32); b_nlam = Buf("nlam")
        mod_sb = sb(top, "mod_sb", [128, 48, 2], F32); b_mod = Buf("mod")
        Ga = sb(top, "Ga", [128, 8], F32); Gca = sb(top, "Gca", [128, 8], F32)
        SHa = sb(top, "SHa", [128, 8], F32); SHca = sb(top, "SHca", [128, 8], F32)
        Gm = sb(top, "Gm", [128, 8], F32); SHm = sb(top, "SHm", [128, 8], F32)
        gAv = sb(top, "gAv", [128, 8], F32); gMv = sb(top, "gMv", [128, 8], F32)
        b_vecs = Buf("vecs")
        b_bc = Buf("bcast")
        QT = sb(top, "QT", [128, 8, OWN], BF16); b_QT = Buf("QT")
        QTf = QT.rearrange("p c t -> p (c t)").bitcast(F32)
        gA_b, gM_b, GmB, SHmB = (QTf[:, i * D:(i + 1) * D] for i in range(4))
        yA = sb(top, "yA", [128, NOWN, 512], BF16); yB = sb(top, "yB", [128, NOWN, 512], BF16)
        b_y = [Buf(f"y{t}") for t in range(NOWN)]
        WdR = sb(top, "WdR", [128, NOWN, NE], F32); b_WdR = [Buf(f"wdr{t}") for t in range(NOWN)]

        s01 = contextlib.ExitStack()
        Wkvq = sb(s01, "Wkvq", [128, 8, 2304], BF16); b_Wkvq = Buf("Wkvq")
        ph0 = contextlib.ExitStack()
        with ph0:
            P.dma_group("wkvq", [("pool", (lambda e, k=k: e.dma_start(out=Wkvq[:, k, :], in_=w_in[k * 128:(k + 1) * 128, 0:2304])), [b_Wkvq])
                                 for k in range(8)])

            for t_, b_ in ((ident_bf, b_identbf), (ident32, b_ident32)):
                P.op("pool", lambda e, t_=t_: e.memset(t_, 1.0), writes=[b_])
                P.op("pool", lambda e, t_=t_: e.affine_select(out=t_, in_=t_, pattern=[[-1, 128]], compare_op=ALU.is_equal,
                                                              fill=0.0, base=0, channel_multiplier=1), reads=[b_], writes=[b_])
            P.op("dve", lambda e: e.memset(ones32, 1.0), writes=[b_ones32])
            P.op("dve", lambda e: e.memset(ones_bf, 1.0), writes=[b_onesbf])
            cgrp = [("sp", lambda e: e.dma_start(out=rb_b, in_=router_b.partition_broadcast(128)), [b_rbb])]
            for i in range(4):
                cgrp.append(("sp", lambda e, i=i: e.dma_start(out=gains[:, i, :], in_=qk_gain[i].partition_broadcast(128)), [b_gains]))
            lamt = sb(ph0, "lamt", [128, 4, 64], F32); b_lamt = Buf("lamt")
            lamp = sb(ph0, "lamp", [128, 2, 64], F32); b_lamp = Buf("lamp")
            lams = sb(ph0, "lams", [128, 2], F32); b_lams = Buf("lams")
            lame = sb(ph0, "lame", [128, 2], F32); b_lame = Buf("lame")
            subt = sb(ph0, "subt", [128, 128], F32); b_subt = Buf("subt")
            for i in range(4):
                cgrp.append(("sp", lambda e, i=i: e.dma_start(out=lamt[:, i, :], in_=lam_v[i].partition_broadcast(128)), [b_lamt]))
            cgrp.append(("sp", lambda e: e.dma_start(out=subt, in_=subln_d.partition_broadcast(128)), [b_subt]))
            cv = sb(ph0, "cv", [128, 16], F32); b_cv = Buf("cv")
            cvs = sb(ph0, "cvs", [128, 16], F32); b_cvs = Buf("cvs")
            bal = sb(ph0, "bal", [128, 48], F32); b_bal = Buf("bal")
            nal = sb(ph0, "nal", [128, 8], F32); nml = sb(ph0, "nml", [128, 8], F32); b_nl = Buf("nl")
            cgrp.append(("sp", lambda e: e.dma_start(out=cv, in_=cvec_d), [b_cv]))
            cgrp.append(("sp", lambda e: e.dma_start(out=bal, in_=b_ada_l), [b_bal]))
            cgrp.append(("sp", lambda e: e.dma_start(out=nal, in_=nrm_a_l), [b_nl]))
            cgrp.append(("sp", lambda e: e.dma_start(out=nml, in_=nrm_m_l), [b_nl]))
            P.dma_group("c", cgrp)
            P.op("dve", lambda e: e.tensor_scalar(out=gsub, in0=subt, scalar1=1.0 - LAM_INIT, scalar2=None, op0=ALU.mult),
                 reads=[b_subt], writes=[b_gsub])
            lam4 = lamt.rearrange("p (a b) d -> p a b d", b=2)
            P.op("dve", lambda e: e.tensor_tensor(out=lamp, in0=lam4[:, :, 0, :], in1=lam4[:, :, 1, :], op=ALU.mult),
                 reads=[b_lamt], writes=[b_lamp])
            P.op("dve", lambda e: e.tensor_reduce(out=lams, in_=lamp, axis=AX.X, op=ALU.add), reads=[b_lamp], writes=[b_lams])
            P.op("act", lambda e: e.activation(out=lame, in_=lams, func=AF.Exp), reads=[b_lams], writes=[b_lame])
            P.op("dve", lambda e: e.tensor_tensor(out=nlam, in0=lame[:, 1:2], in1=lame[:, 0:1], op=ALU.subtract),
                 reads=[b_lame], writes=[b_nlam])
            P.op("dve", lambda e: e.tensor_scalar(out=nlam, in0=nlam, scalar1=-LAM_INIT, scalar2=None, op0=ALU.add),
                 reads=[b_nlam], writes=[b_nlam])

            P.op("act", lambda e: e.activation(out=cvs, in_=cv, func=AF.Exp, scale=-1.0), reads=[b_cv], writes=[b_cvs])
            P.op("dve", lambda e: e.tensor_scalar(out=cvs, in0=cvs, scalar1=1.0, scalar2=None, op0=ALU.add), reads=[b_cvs], writes=[b_cvs])
            P.op("dve", lambda e: e.reciprocal(out=cvs, in_=cvs), reads=[b_cvs], writes=[b_cvs])
            P.op("dve", lambda e: e.tensor_tensor(out=cvs, in0=cvs, in1=cv, op=ALU.mult), reads=[b_cvs, b_cv], writes=[b_cvs])
            wk_ring = mkring(ph0, "wadak", 2, [128, 8, D], F32)
            modps, b_modps = bank(0)
            modv = modps[:, 0:96].rearrange("p (m w) -> p m w", w=2)
            w_ada_v = w_ada.rearrange("(k p) n -> p k n", p=128)
            for blk in range(6):
                wk, b_wk = wk_ring.next()
                P.op("sp", lambda e, wk=wk, blk=blk: e.dma_start(out=wk, in_=w_ada_v[:, :, blk * D:(blk + 1) * D]),
                     writes=[b_wk], dma=f"wada{blk % 2}")

                def mm(e, wk=wk, blk=blk):
                    ins = None
                    for mm_ in range(8):
                        m = blk * 8 + mm_
                        for k in range(8):
                            ins = e.matmul(modv[:, m, :], lhsT=wk[:, k, mm_ * 128:(mm_ + 1) * 128], rhs=cvs[:, 2 * k:2 * k + 2],
                                           start=(k == 0), stop=(k == 7))
                    return ins
                P.op("pe", mm, reads=[b_wk, b_cvs], writes=[b_modps])
            P.op("dve", lambda e: e.tensor_tensor(out=mod_sb, in0=modv, in1=bal.unsqueeze(2).to_broadcast([128, 48, 2]), op=ALU.add),
                 reads=[b_modps, b_bal], writes=[b_mod])

            def vecs(e):
                e.scalar_tensor_tensor(out=Ga, in0=mod_sb[:, 8:16, 0], scalar=1.0, in1=nal, op0=ALU.add, op1=ALU.mult)
                e.scalar_tensor_tensor(out=Gca, in0=mod_sb[:, 8:16, 1], scalar=1.0, in1=nal, op0=ALU.add, op1=ALU.mult)
                e.scalar_tensor_tensor(out=Gm, in0=mod_sb[:, 32:40, 0], scalar=1.0, in1=nml, op0=ALU.add, op1=ALU.mult)
                e.tensor_copy(out=SHa, in_=mod_sb[:, 0:8, 0])
                e.tensor_copy(out=SHca, in_=mod_sb[:, 0:8, 1])
                e.tensor_copy(out=SHm, in_=mod_sb[:, 24:32, 0])
                e.tensor_copy(out=gAv, in_=mod_sb[:, 16:24, 0])
                return e.tensor_copy(out=gMv, in_=mod_sb[:, 40:48, 0])
            P.op("dve", vecs, reads=[b_mod, b_nl], writes=[b_vecs])
            P.barrier()
            P.emit()

        def norm_to_hT(xt, b_xt, G, SH, bufs):
            junk, b_junk, ss, b_ss, xs, b_xs, hT, b_hT, tp, b_tp = bufs
            P.op("act", lambda e: e.activation(out=junk, in_=xt, func=AF.Square, accum_out=ss), reads=[b_xt], writes=[b_junk, b_ss])
            P.op("act", lambda e: e.activation(out=ss, in_=ss, func=AF.Ln, scale=1.0 / D, bias=EPS), reads=[b_ss], writes=[b_ss])
            P.op("act", lambda e: e.activation(out=ss, in_=ss, func=AF.Exp, scale=-0.5), reads=[b_ss], writes=[b_ss])
            P.op("dve", lambda e: e.tensor_scalar(out=xs, in0=xt, scalar1=ss, scalar2=None, op0=ALU.mult), reads=[b_xt, b_ss], writes=[b_xs])
            tpb = tp.bitcast(BF16).rearrange("p (k t) -> p k t", t=128)

            def tr(e):
                ins = None
                for k in range(8):
                    ins = e.transpose(out=tpb[:, k, :], in_=xs[:, k * 128:(k + 1) * 128], identity=ident_bf)
                return ins
            P.op("pe", tr, reads=[b_xs, b_identbf], writes=[b_tp])

            def ev(e):
                ins = None
                for k in range(8):
                    ins = e.activation(out=hT[:, k, :], in_=tpb[:, k, :], func=AF.Identity, scale=G[:, k:k + 1], bias=SH[:, k:k + 1])
                return ins
            P.op("act", ev, reads=[b_tp, b_vecs], writes=[b_hT])

        ph1 = contextlib.ExitStack()
        with ph1:
            def lanes(name, n, shape, dt):
                return [mkring(ph1, f"{name}L{l}_", n, shape, dt) for l in range(2)]
            XT = lanes("xt", 2, [128, D], F32)
            RT = lanes("rt", 2, [128, 128], F32)
            JK = mkring(ph1, "junk", 1, [128, D], BF16)
            SS = lanes("ss", 1, [128, 1], F32)
            XS = lanes("xs", 1, [128, D], BF16)
            HT = lanes("hT", 1, [128, 8, 128], BF16)
            SQ = lanes("sq", 2, [128, 512], F32)
            S8 = lanes("s8", 2, [128, 8], F32)
            XG = lanes("xg", 2, [128, 512], F32)
            RA = lanes("ra", 2, [128, 512], F32)
            RB = lanes("rbm", 2, [128, 512], F32)
            KTM = lanes("ktm", 1, [128, 640], BF16)
            QTM = lanes("qtm", 1, [128, 1024], BF16)
            KG = mkring(ph1, "kg", 2, [128, 5, 512], BF16)
            VG = mkring(ph1, "vg", 2, [128, 5, 4 * 130], BF16)
            for (vg, b_vg) in VG.items:
                P.op("pool", lambda e, vg=vg: e.memset(vg, 1.0), writes=[b_vg])
            TPB = [Ring([bank(0)]), Ring([bank(1)])]
            PJ = [Ring([bank(2), bank(3)]), Ring([bank(4), bank(5)])]
            KTP = [Ring([bank(6)]), Ring([bank(7)])]

            def normrope(ln, ps, b_ps, H, gi, rt, b_rt, out4, b_out, perm=None):
                W = H * 64
                sq, b_sq = SQ[ln].next(); s8, b_s8 = S8[ln].next(); xg, b_xg = XG[ln].next()
                v3 = lambda a: a[:, 0:W].rearrange("p (h d) -> p h d", d=64)
                P.op("act", lambda e: e.activation(out=sq[:, 0:W], in_=ps, func=AF.Square), reads=[b_ps], writes=[b_sq])
                P.op("dve", lambda e: e.tensor_reduce(out=s8[:, 0:H], in_=v3(sq), axis=AX.X, op=ALU.add), reads=[b_sq], writes=[b_s8])
                P.op("act", lambda e: e.activation(out=s8[:, 0:H], in_=s8[:, 0:H], func=AF.Ln, scale=1.0 / 64, bias=EPS), reads=[b_s8], writes=[b_s8])
                P.op("act", lambda e: e.activation(out=s8[:, 0:H], in_=s8[:, 0:H], func=AF.Exp, scale=-0.5), reads=[b_s8], writes=[b_s8])
                g = gains[:, gi, :]
                P.op("dve", lambda e: e.tensor_tensor(out=v3(xg), in0=ps.rearrange("p (h d) -> p h d", d=64),
                                                      in1=g.unsqueeze(1).to_broadcast([128, H, 64]), op=ALU.mult),
                     reads=[b_ps, b_gains], writes=[b_xg])
                src, b_src = xg, b_xg
                if rt is not None:
                    ra, b_ra = RA[ln].next(); rbm, b_rbm = RB[ln].next()
                    cosF = rt[:, 0:64]
                    sinS = rt[:, 64:128].rearrange("p (i two) -> p i two", two=2)
                    P.op("dve", lambda e: e.tensor_tensor(out=v3(ra), in0=v3(xg), in1=cosF.unsqueeze(1).to_broadcast([128, H, 64]), op=ALU.mult),
                         reads=[b_xg, b_rt], writes=[b_ra])
                    x4 = xg[:, 0:W].rearrange("p (h i two) -> p h i two", i=32, two=2)
                    r4 = rbm[:, 0:W].rearrange("p (h i two) -> p h i two", i=32, two=2)

                    def sw(e):
                        e.tensor_tensor(out=r4[:, :, :, 0], in0=x4[:, :, :, 1], in1=sinS[:, :, 0].unsqueeze(1).to_broadcast([128, H, 32]), op=ALU.mult)
                        return e.tensor_tensor(out=r4[:, :, :, 1], in0=x4[:, :, :, 0], in1=sinS[:, :, 1].unsqueeze(1).to_broadcast([128, H, 32]), op=ALU.mult)
                    P.op("dve", sw, reads=[b_xg, b_rt], writes=[b_rbm])
                    P.op("dve", lambda e: e.tensor_tensor(out=ra[:, 0:W], in0=ra[:, 0:W], in1=rbm[:, 0:W], op=ALU.add),
                         reads=[b_ra, b_rbm], writes=[b_ra])
                    src, b_src = ra, b_ra
                if perm is None:
                    P.op("dve", lambda e: e.tensor_tensor(out=out4, in0=v3(src), in1=s8[:, 0:H].unsqueeze(2).to_broadcast([128, H, 64]), op=ALU.mult),
                         reads=[b_src, b_s8], writes=[b_out])
                else:
                    s4 = src[:, 0:W].rearrange("p (g r d) -> p g r d", g=2, d=64)
                    r4b = s8[:, 0:H].rearrange("p (g r) -> p g r", g=2).unsqueeze(3).to_broadcast([128, 2, 4, 64])
                    P.op("dve", lambda e: e.tensor_tensor(out=out4, in0=s4, in1=r4b, op=ALU.mult), reads=[b_src, b_s8], writes=[b_out])

            def proj(ln, hT, b_hT, c0, c1):
                ps, b_ps = PJ[ln].next()
                W = c1 - c0

                def mm(e):
                    ins = None
                    for k in range(8):
                        ins = e.matmul(ps[:, 0:W], lhsT=hT[:, k, :], rhs=Wkvq[:, k, c0:c1], start=(k == 0), stop=(k == 7))
                    return ins
                P.op("pe", mm, reads=[b_hT, b_Wkvq], writes=[b_ps])
                return ps, b_ps

            def front(ti):
                is_ctx = ti >= 64
                ln = ti % 2
                xt, b_xt = XT[ln].next()
                src = ctx_d[(ti - 64) * 128:(ti - 63) * 128, :] if is_ctx else xp[ti * 128:(ti + 1) * 128, :]
                P.op("sp", lambda e, xt=xt, src=src: e.dma_start(out=xt, in_=src), writes=[b_xt], dma=f"xt{ln}_{(XT[ln].i - 1) % 2}")
                rt, b_rt = (None, None)
                if not is_ctx:
                    rt, b_rt = RT[ln].next()
                    P.op("sp", lambda e, rt=rt, ti=ti: e.dma_start(out=rt, in_=rope_d[ti * 128:(ti + 1) * 128, :]),
                         writes=[b_rt], dma=f"rt{ln}_{(RT[ln].i - 1) % 2}")
                junk, b_junk = JK.next(); ss, b_ss = SS[ln].next(); xs, b_xs = XS[ln].next(); hT, b_hT = HT[ln].next(); tp, b_tp = TPB[ln].next()
                norm_to_hT(xt, b_xt, Gca if is_ctx else Ga, SHca if is_ctx else SHa,
                           (junk, b_junk, ss, b_ss, xs, b_xs, hT, b_hT, tp, b_tp))
                if DEBUG and ti == 0:
                    P.op("sp", lambda e, hT=hT: e.dma_start(out=dbg_hT, in_=hT.rearrange("p k t -> p (k t)")), reads=[b_hT], dma="dbg1")
                    P.op("sp", lambda e, xs=xs: e.dma_start(out=dbg_xs, in_=xs), reads=[b_xs], dma="dbg1")
                    P.op("sp", lambda e: e.dma_start(out=dbg_mod, in_=mod_sb.rearrange("p m w -> p (m w)")), reads=[b_mod], dma="dbg1")
                    P.op("sp", lambda e: e.dma_start(out=dbg_vec[:, 0:8], in_=Ga), reads=[b_vecs], dma="dbg1")
                    P.op("sp", lambda e: e.dma_start(out=dbg_vec[:, 8:16], in_=SHa), reads=[b_vecs], dma="dbg1")
                return hT, b_hT, rt, b_rt

            ngrp = (NKT + 3) // 4
            traces1 = []
            for grp in range(ngrp):
                t0 = grp * 4
                nt = min(4, NKT - t0)
                kg, b_kg = KG.next(); vg, b_vg = VG.next()
                vg4 = vg.rearrange("p s (t c) -> p s t c", c=130)
                for tg in range(nt):
                  def tile_body(grp=grp, t0=t0, nt=nt, kg=kg, b_kg=b_kg, vg=vg, b_vg=b_vg, vg4=vg4, tg=tg):
                    ti = t0 + tg
                    is_ctx = ti >= 64
                    own = ti < NOWN
                    hT, b_hT, rt, b_rt = front(ti)
                    ln = ti % 2
                    ktm, b_ktm = KTM[ln].next()
                    ps, b_ps = proj(ln, hT, b_hT, 512, 768)
                    normrope(ln, ps[:, 0:128], b_ps, 2, 1, rt, b_rt, ktm[:, 0:128].rearrange("p (h d) -> p h d", d=64), b_ktm)
                    vA = vg4[:, 0, tg, :].rearrange("p (m d) -> p m d", d=65)[:, :, 0:64]
                    P.op("act", lambda e, vA=vA, ps=ps: e.activation(out=vA, in_=ps[:, 128:256].rearrange("p (m d) -> p m d", d=64), func=AF.Copy),
                         reads=[b_ps], writes=[b_vg])
                    ps, b_ps = proj(ln, hT, b_hT, 1280, 1792)
                    normrope(ln, ps, b_ps, 8, 3, rt, b_rt, ktm[:, 128:640].rearrange("p (h d) -> p h d", d=64), b_ktm)
                    ps, b_ps = proj(ln, hT, b_hT, 1792, 2304)
                    vB4 = vg4[:, 1:5, tg, 0:128]
                    P.op("act", lambda e, vB4=vB4, ps=ps: e.activation(out=vB4, in_=ps.rearrange("p (s d) -> p s d", d=128), func=AF.Copy),
                         reads=[b_ps], writes=[b_vg])
                    kp, b_kp = KTP[ln].next()
                    kpb = kp.bitcast(BF16).rearrange("p (c t) -> p c t", t=128)

                    def ktr(e, ktm=ktm, kpb=kpb):
                        ins = None
                        for c in range(5):
                            ins = e.transpose(out=kpb[:, c, :], in_=ktm[:, c * 128:(c + 1) * 128], identity=ident_bf)
                        return ins
                    P.op("pe", ktr, reads=[b_ktm, b_identbf], writes=[b_kp])
                    P.op("dve", lambda e, kg=kg, kpb=kpb, tg=tg: e.tensor_copy(out=kg[:, :, tg * 128:(tg + 1) * 128], in_=kpb[:, 0:5, :]),
                         reads=[b_kp], writes=[b_kg])
                    if own:
                        qtm, b_qtm = QTM[ln].next()
                        ps, b_ps = proj(ln, hT, b_hT, 0, 512)
                        normrope(ln, ps, b_ps, 8, 0, rt, b_rt, qtm[:, 0:512].rearrange("p (r g d) -> p g r d", g=2, d=64), b_qtm, perm=True)
                        ps, b_ps = proj(ln, hT, b_hT, 768, 1280)
                        normrope(ln, ps, b_ps, 8, 2, rt, b_rt, qtm[:, 512:1024].rearrange("p (h d) -> p h d", d=64), b_qtm)
                        kp, b_kp = KTP[ln].next()
                        qpb = kp.bitcast(BF16).rearrange("p (c t) -> p c t", t=128)

                        def qtr(e, qtm=qtm, qpb=qpb):
                            ins = None
                            for c in range(8):
                                ins = e.transpose(out=qpb[:, c, :], in_=qtm[:, c * 128:(c + 1) * 128], identity=ident_bf)
                            return ins
                        P.op("pe", qtr, reads=[b_qtm, b_identbf], writes=[b_kp])
                        P.op("act", lambda e, qpb=qpb, ti=ti: e.activation(out=QT[:, :, ti * 128:(ti + 1) * 128], in_=qpb, func=AF.Copy),
                             reads=[b_kp], writes=[b_QT])
                  def stores(grp=grp, t0=t0, nt=nt, kg=kg, b_kg=b_kg, vg=vg, b_vg=b_vg):
                    P.op("sp", lambda e: e.dma_start(out=KTs[:, :, t0 * 128:(t0 + nt) * 128].rearrange("c p t -> p c t"), in_=kg[:, :, 0:nt * 128]),
                         reads=[b_kg], writes=[b_KTs], dma=f"kst{grp % 2}")
                    P.op("sp", lambda e: e.dma_start(out=Vs[:, :, t0 * 130:(t0 + nt) * 130].rearrange("c p t -> p c t"), in_=vg[:, :, 0:nt * 130]),
                         reads=[b_vg], writes=[b_Vs], dma=f"vst{grp % 2}")
                  traces1.append((P.record(tile_body), P.record(stores) if tg == nt - 1 else []))
            P.replay(traces1, width=2)
            if DEBUG:
                P.op("sp", lambda e: e.dma_start(out=dbg_QT, in_=QT.rearrange("p c t -> p (c t)")), reads=[b_QT], dma="dbg1")
            P.barrier()
            P.emit()

        s01.close()
        if STOP_AFTER <= 1:
            return nc
        ph2 = contextlib.ExitStack()
        with ph2:
            KV = [(sb(ph2, f"KT{i}", [128, NKEY], BF16), sb(ph2, f"V{i}", [128, NKT, 130], BF16), Buf(f"kv{i}")) for i in range(2)]
            PB = mkring(ph2, "pb", 6, [128, 1024], BF16)
            OS = mkring(ph2, "osb", 1, [65, 1024], F32)
            RD = mkring(ph2, "rden", 2, [128, 2], F32)
            NL = mkring(ph2, "nl", 2, [128, 1], F32)
            O1 = mkring(ph2, "o1", 2, [128, 128], F32)
            OO = mkring(ph2, "oo", 2, [128, 128], F32)
            JS = mkring(ph2, "js", 1, [128, 128], F32)
            SSB = mkring(ph2, "ssb", 2, [128, 1], F32)
            PP2 = mkring(ph2, "pp2", 2, [128, 1024], BF16)
            ACCD = [(sb(ph2, f"accd{i}", [128, 1024], F32), Buf(f"accd{i}")) for i in range(2)]
            OSB = mkring(ph2, "osbB", 1, [128, 1024], F32)
            SR = Ring([(DB[0], DBb[0]), (DB[1], DBb[1])])

            def load_kv(L):
                KTt, Vt, b_kv = KV[L % 2]
                P.op("sp", lambda e: e.dma_start(out=KTt, in_=KTs[L]), reads=[b_KTs], writes=[b_kv], dma=f"kvk{L % 2}")
                P.op("sp", lambda e: e.dma_start(out=Vt.rearrange("p t c -> p (t c)"), in_=Vs[L]), reads=[b_Vs], writes=[b_kv], dma=f"kvv{L % 2}")

            load_kv(0)
            load_kv(1)
            fin_i = 0
            blk_i = 0
            pending = []

            def drain(n):
                for _ in range(n):
                    if pending:
                        P.op(*pending.pop(0))
            for u in range(8):
                isB = u >= 4
                L = 0 if not isB else 1 + (u - 4)
                KTt, Vt, b_kv = KV[L % 2]
                for qb in range(4):
                    q0 = qb * 512
                    accd, b_accd = ACCD[blk_i % 2]; blk_i += 1

                    def qk(kt):
                        S, (bs0, bs1) = SR.next()

                        def f(e, S=S, kt=kt, KTt=KTt, u=u, q0=q0):
                            e.matmul(S[:, 0:512], lhsT=KTt[0:64, kt * 128:(kt + 1) * 128], rhs=QT[0:64, u, q0:q0 + 512], start=True, stop=True)
                            return e.matmul(S[:, 512:1024], lhsT=KTt[64:128, kt * 128:(kt + 1) * 128], rhs=QT[64:128, u, q0:q0 + 512], start=True, stop=True)
                        P.op("pe", f, reads=[b_kv, b_QT], writes=[bs0, bs1])
                        return S, bs0, bs1

                    sq_ = [qk(0), qk(1)]
                    for kt in range(NKT):
                        if kt >= 2:
                            drain(2)
                        S, bs0, bs1 = sq_.pop(0)
                        pb, b_pb = PB.next()
                        P.op("act", lambda e, pb=pb, S=S: e.activation(out=pb, in_=S, func=AF.Exp, scale=0.125), reads=[bs0, bs1], writes=[b_pb])
                        if kt + 2 < NKT:
                            sq_.append(qk(kt + 2))
                        st, sp_ = (kt == 0), (kt == NKT - 1)
                        if not isB:
                            def pv(e, pb=pb, kt=kt, st=st, sp_=sp_, Vt=Vt):
                                e.matmul(DB[2][0:65, 0:512], lhsT=Vt[:, kt, 0:65], rhs=pb[:, 0:512], start=st, stop=sp_)
                                return e.matmul(DB[2][0:65, 512:1024], lhsT=Vt[:, kt, 65:130], rhs=pb[:, 512:1024], start=st, stop=sp_)
                            P.op("pe", pv, reads=[b_pb, b_kv], writes=[DBb[2][0], DBb[2][1]])
                        else:
                            def pv(e, pb=pb, kt=kt, st=st, sp_=sp_, Vt=Vt):
                                e.matmul(DB[2][:, 0:512], lhsT=Vt[:, kt, 0:128], rhs=pb[:, 0:512], start=st, stop=sp_)
                                return e.matmul(DB[2][:, 512:1024], lhsT=Vt[:, kt, 0:128], rhs=pb[:, 512:1024], start=st, stop=sp_)
                            P.op("pe", pv, reads=[b_pb, b_kv], writes=[DBb[2][0], DBb[2][1]])
                            if kt % 2 == 0:
                                pb_prev, b_pb_prev = pb, b_pb
                            else:
                                pp2, b_pp2 = PP2.next()
                                P.op("dve", lambda e, pp2=pp2, pb=pb, pb_prev=pb_prev: e.tensor_tensor(out=pp2, in0=pb, in1=pb_prev, op=ALU.add),
                                     reads=[b_pb, b_pb_prev], writes=[b_pp2])
                                if kt == 1:
                                    P.op("dve", lambda e, pp2=pp2, accd=accd: e.tensor_copy(out=accd, in_=pp2), reads=[b_pp2], writes=[b_accd])
                                else:
                                    P.op("dve", lambda e, pp2=pp2, accd=accd: e.tensor_tensor(out=accd, in0=accd, in1=pp2, op=ALU.add), reads=[b_pp2, b_accd], writes=[b_accd])
                    drain(len(pending))
                    if not isB:
                        os2, b_os2 = OS.next()
                        P.op("dve", lambda e, os2=os2: e.tensor_copy(out=os2, in_=DB[2][0:65, :]), reads=[DBb[2][0], DBb[2][1]], writes=[b_os2])
                    else:
                        osB, b_osB = OSB.next()
                        P.op("dve", lambda e, osB=osB: e.tensor_copy(out=osB, in_=DB[2]), reads=[DBb[2][0], DBb[2][1]], writes=[b_osB])
                    P.rec = []
                    for qi in range(4):
                        qt = qb * 4 + qi
                        fb, b_fb = bank(6 + fin_i % 2); fin_i += 1
                        c0 = qi * 128
                        if not isB:
                            F3 = fb[:, 0:260].rearrange("p (m c) -> p m c", c=130)

                            def ftr(e, os2=os2, F3=F3, c0=c0):
                                e.transpose(out=F3[:, 0, 0:65], in_=os2[0:65, c0:c0 + 128], identity=ident32[0:65, 0:65])
                                return e.transpose(out=F3[:, 1, 0:65], in_=os2[0:65, 512 + c0:512 + c0 + 128], identity=ident32[0:65, 0:65])
                            P.op("pe", ftr, reads=[b_os2, b_ident32], writes=[b_fb])
                            rd, b_rd = RD.next()
                            P.op("dve", lambda e, rd=rd, F3=F3: e.reciprocal(out=rd, in_=F3[:, :, 64]), reads=[b_fb], writes=[b_rd])
                            yo = yA[:, qt, :].rearrange("p (m r d) -> p m r d", m=2, d=64)[:, :, u, :]
                            P.op("dve", lambda e, rd=rd, F3=F3, yo=yo: e.tensor_tensor(out=yo, in0=F3[:, :, 0:64], in1=rd.unsqueeze(2).to_broadcast([128, 2, 64]), op=ALU.mult),
                                 reads=[b_fb, b_rd], writes=[b_y[qt]])
                        else:
                            def ftr(e, osB=osB, fb=fb, c0=c0, accd=accd):
                                e.transpose(out=fb[:, 0:128], in_=osB[:, c0:c0 + 128], identity=ident32)
                                e.transpose(out=fb[:, 128:256], in_=osB[:, 512 + c0:512 + c0 + 128], identity=ident32)
                                e.matmul(fb[:, 256:257], lhsT=accd[:, c0:c0 + 128], rhs=ones32[:, 0:1], start=True, stop=True)
                                return e.matmul(fb[:, 257:258], lhsT=accd[:, 512 + c0:512 + c0 + 128], rhs=ones32[:, 0:1], start=True, stop=True)
                            P.op("pe", ftr, reads=[b_osB, b_ident32, b_accd, b_ones32], writes=[b_fb])
                            rd, b_rd = RD.next(); nl, b_nl = NL.next(); o1, b_o1 = O1.next(); oo, b_oo = OO.next()
                            js, b_js = JS.next(); ssb, b_ssb = SSB.next()
                            P.op("dve", lambda e, rd=rd, fb=fb: e.reciprocal(out=rd, in_=fb[:, 256:258]), reads=[b_fb], writes=[b_rd])
                            P.op("dve", lambda e, rd=rd, nl=nl: e.tensor_tensor(out=nl, in0=rd[:, 1:2], in1=nlam, op=ALU.mult), reads=[b_rd, b_nlam], writes=[b_nl])
                            P.op("dve", lambda e, o1=o1, fb=fb, rd=rd: e.tensor_scalar(out=o1, in0=fb[:, 0:128], scalar1=rd[:, 0:1], scalar2=None, op0=ALU.mult),
                                 reads=[b_fb, b_rd], writes=[b_o1])
                            P.op("dve", lambda e, oo=oo, o1=o1, fb=fb, nl=nl: e.scalar_tensor_tensor(
                                out=oo, in0=fb[:, 128:256], scalar=nl, in1=o1, op0=ALU.mult, op1=ALU.add),
                                reads=[b_fb, b_nl, b_o1], writes=[b_oo])
                            P.op("act", lambda e, js=js, oo=oo, ssb=ssb: e.activation(out=js, in_=oo, func=AF.Square, accum_out=ssb), reads=[b_oo], writes=[b_js, b_ssb])
                            P.op("act", lambda e, ssb=ssb: e.activation(out=ssb, in_=ssb, func=AF.Ln, scale=1.0 / 128, bias=SUBLN_EPS), reads=[b_ssb], writes=[b_ssb])
                            P.op("act", lambda e, ssb=ssb: e.activation(out=ssb, in_=ssb, func=AF.Exp, scale=-0.5), reads=[b_ssb], writes=[b_ssb])
                            hb = u - 4
                            P.op("dve", lambda e, oo=oo, ssb=ssb, qt=qt, hb=hb: e.scalar_tensor_tensor(
                                out=yB[:, qt, hb * 128:(hb + 1) * 128], in0=oo, scalar=ssb, in1=gsub, op0=ALU.mult, op1=ALU.mult),
                                reads=[b_oo, b_ssb, b_gsub], writes=[b_y[qt]])
                    pending.extend(P.rec); P.rec = None
                if u == 3:
                    load_kv(2)
                elif 4 <= u < 6:
                    load_kv(u - 4 + 3)
            drain(len(pending))
            if DEBUG:
                for t in range(NOWN):
                    P.op("sp", lambda e, t=t: e.dma_start(out=Ydbg[t * 128:(t + 1) * 128, 0:512], in_=yA[:, t, :]), reads=[b_y[t]], dma="dbgy")
                    P.op("sp", lambda e, t=t: e.dma_start(out=Ydbg[t * 128:(t + 1) * 128, 512:1024], in_=yB[:, t, :]), reads=[b_y[t]], dma="dbgy")
            P.barrier()
            P.emit()

        if STOP_AFTER <= 2:
            return nc
        ph3 = contextlib.ExitStack()
        with ph3:
            Wg_ = sb(ph3, "Wgate", [128, 8, 2048], BF16); b_Wg_ = Buf("Wgate")
            Woa = sb(ph3, "Woa", [128, 4, D], BF16); Wob = sb(ph3, "Wob", [128, 4, D], BF16); b_Wo = Buf("Wo")
            Wout = sb(ph3, "Wout", [128, 8, D], BF16); b_Wout = Buf("Wout")
            rw32 = sb(ph3, "rw32", [128, 8, NE], F32); b_rw = Buf("rw")
            P.dma_group("w3a", [("pool", (lambda e, k=k: e.dma_start(out=Wg_[:, k, :], in_=w_in[k * 128:(k + 1) * 128, 2304:4352])), [b_Wg_])
                                for k in range(8)])
            w3 = []
            for k in range(4):
                w3.append(("pool", (lambda e, k=k: e.dma_start(out=Woa[:, k, :], in_=w_oa[k * 128:(k + 1) * 128, :])), [b_Wo]))
                w3.append(("pool", (lambda e, k=k: e.dma_start(out=Wob[:, k, :], in_=w_ob[k * 128:(k + 1) * 128, :])), [b_Wo]))
            for k in range(8):
                w3.append(("pool", (lambda e, k=k: e.dma_start(out=Wout[:, k, :], in_=w_out[k * 128:(k + 1) * 128, :])), [b_Wout]))
            P.dma_group("w3", w3)
            P.op("sp", lambda e: e.dma_start(out=rw32, in_=router_w.rearrange("(k p) n -> p k n", p=128)), writes=[b_rw], dma="rw")
            dg_ring = mkring(ph3, "dg", 2, [128, 128], F32)
            bi = 1
            for (vec, dst) in ((gAv, gA_b), (gMv, gM_b), (Gm, GmB), (SHm, SHmB)):
                for k in range(8):
                    dg, b_dg = dg_ring.next()
                    pb, b_pb = bank(1 + (bi % 2)); bi += 1
                    P.op("dve", lambda e, dg=dg, vec=vec, k=k: e.tensor_scalar(out=dg, in0=ident32, scalar1=vec[:, k:k + 1], scalar2=None, op0=ALU.mult),
                         reads=[b_ident32, b_vecs], writes=[b_dg])
                    P.op("pe", lambda e, dg=dg, pb=pb: e.matmul(pb[:, 0:128], lhsT=ones32, rhs=dg, start=True, stop=True),
                         reads=[b_ones32, b_dg], writes=[b_pb])
                    P.op("act", lambda e, dst=dst, pb=pb, k=k: e.activation(out=dst[:, k * 128:(k + 1) * 128], in_=pb[:, 0:128], func=AF.Copy),
                         reads=[b_pb], writes=[b_bc])
            XT = mkring(ph3, "xt3", 2, [128, D], F32)
            JK = mkring(ph3, "junk3", 1, [128, D], BF16)
            SS = mkring(ph3, "ss3", 4, [128, 1], F32)
            XS = mkring(ph3, "xs3", 2, [128, D], BF16)
            HT = mkring(ph3, "hT3", 2, [128, 8, 128], BF16)
            SG = mkring(ph3, "sg", 2, [128, 2048], F32)
            YT = mkring(ph3, "yT", 1, [128, 8, 128], BF16)
            M1 = mkring(ph3, "m1", 1, [128, D], F32)
            MG = mkring(ph3, "mg", 1, [128, D], BF16)
            MT = mkring(ph3, "mT", 1, [128, 8, 128], BF16)
            X1 = mkring(ph3, "x1", 2, [128, D], F32)
            XS2 = mkring(ph3, "xs2", 1, [128, D], F32)
            H2B = mkring(ph3, "h2b", 2, [128, HW_], BF16)
            H2T = mkring(ph3, "h2T", 1, [128, 8, 128], F32)
            LG = mkring(ph3, "lg", 2, [128, NE], F32)
            M8 = mkring(ph3, "m8", 2, [128, 8], F32)
            MK = mkring(ph3, "mk", 2, [128, NE], F32)
            EX = mkring(ph3, "ex", 2, [128, NE], F32)
            SM = mkring(ph3, "sm", 2, [128, 2], F32)

            def stage1(t):
                xt, b_xt = XT.next()
                P.op("sp", lambda e, xt=xt, t=t: e.dma_start(out=xt, in_=xp[t * 128:(t + 1) * 128, :]), writes=[b_xt], dma=f"xt3{t % 2}")
                junk, b_junk = JK.next(); ss, b_ss = SS.next(); xs, b_xs = XS.next(); hT, b_hT = HT.next()
                tp, b_tp = bank(0)
                norm_to_hT(xt, b_xt, Ga, SHa, (junk, b_junk, ss, b_ss, xs, b_xs, hT, b_hT, tp, b_tp))
                sg, b_sg = SG.next()
                for gb in range(4):
                    ps, b_ps = bank(2 + gb % 2)

                    def mm(e, ps=ps, gb=gb, hT=hT):
                        ins = None
                        for k in range(8):
                            ins = e.matmul(ps, lhsT=hT[:, k, :], rhs=Wg_[:, k, gb * 512:(gb + 1) * 512], start=(k == 0), stop=(k == 7))
                        return ins
                    P.op("pe", mm, reads=[b_hT, b_Wg_], writes=[b_ps])
                    sgs = sg[:, gb * 512:(gb + 1) * 512]
                    P.op("act", lambda e, sgs=sgs, ps=ps: e.activation(out=sgs, in_=ps, func=AF.Sigmoid), reads=[b_ps], writes=[b_sg])
                return xt, b_xt, sg, b_sg

            st1 = {0: stage1(0)}
            x1s = {}

            def s2(t, xt, b_xt, sg, b_sg):
                yT, b_yT = YT.next()
                tp, b_tp = bank(1)
                tpb = tp.bitcast(BF16).rearrange("p (k t) -> p k t", t=128)

                def ytr(e, tpb=tpb, t=t):
                    ins = None
                    for k in range(4):
                        e.transpose(out=tpb[:, k, :], in_=yA[:, t, k * 128:(k + 1) * 128], identity=ident_bf)
                        ins = e.transpose(out=tpb[:, 4 + k, :], in_=yB[:, t, k * 128:(k + 1) * 128], identity=ident_bf)
                    return ins
                P.op("pe", ytr, reads=[b_y[t], b_identbf], writes=[b_tp])
                P.op("dve", lambda e, yT=yT, tpb=tpb: e.tensor_copy(out=yT, in_=tpb), reads=[b_tp], writes=[b_yT])
                m1, b_m1 = M1.next(); mg, b_mg = MG.next()
                for cb in range(2):
                    pa, b_pa = bank(4 + cb); pbk, b_pbk = bank(6 + cb)

                    def mmo(e, pa=pa, pbk=pbk, cb=cb, yT=yT):
                        for k in range(4):
                            e.matmul(pa, lhsT=yT[:, k, :], rhs=Woa[:, k, cb * 512:(cb + 1) * 512], start=(k == 0), stop=(k == 3))
                        ins = None
                        for k in range(4):
                            ins = e.matmul(pbk, lhsT=yT[:, 4 + k, :], rhs=Wob[:, k, cb * 512:(cb + 1) * 512], start=(k == 0), stop=(k == 3))
                        return ins
                    P.op("pe", mmo, reads=[b_yT, b_Wo], writes=[b_pa, b_pbk])
                    cs = slice(cb * 512, (cb + 1) * 512)
                    P.op("dve", lambda e, m1=m1, pa=pa, sg=sg, cs=cs: e.tensor_tensor(out=m1[:, cs], in0=pa, in1=sg[:, cs], op=ALU.mult),
                         reads=[b_pa, b_sg], writes=[b_m1])
                    P.op("dve", lambda e, sg=sg, pbk=pbk, cb=cb: e.tensor_tensor(out=sg[:, 1024 + cb * 512:1024 + (cb + 1) * 512], in0=pbk, in1=sg[:, 1024 + cb * 512:1024 + (cb + 1) * 512], op=ALU.mult),
                         reads=[b_pbk, b_sg], writes=[b_sg])
                    P.op("dve", lambda e, mg=mg, m1=m1, sg=sg, cs=cs, cb=cb: e.tensor_tensor(out=mg[:, cs], in0=m1[:, cs], in1=sg[:, 1024 + cb * 512:1024 + (cb + 1) * 512], op=ALU.add),
                         reads=[b_m1, b_sg], writes=[b_mg])
                mT, b_mT = MT.next()
                tp, b_tp = bank(1)
                tpb = tp.bitcast(BF16).rearrange("p (k t) -> p k t", t=128)

                def mtr(e, tpb=tpb, mg=mg):
                    ins = None
                    for k in range(8):
                        ins = e.transpose(out=tpb[:, k, :], in_=mg[:, k * 128:(k + 1) * 128], identity=ident_bf)
                    return ins
                P.op("pe", mtr, reads=[b_mg, b_identbf], writes=[b_tp])
                P.op("act", lambda e, mT=mT, tpb=tpb: e.activation(out=mT, in_=tpb, func=AF.Copy), reads=[b_tp], writes=[b_mT])
                x1, b_x1 = X1.next()
                for cb in range(2):
                    ps, b_ps = bank(2 + cb)

                    def mmw(e, ps=ps, cb=cb, mT=mT):
                        ins = None
                        for k in range(8):
                            ins = e.matmul(ps, lhsT=mT[:, k, :], rhs=Wout[:, k, cb * 512:(cb + 1) * 512], start=(k == 0), stop=(k == 7))
                        return ins
                    P.op("pe", mmw, reads=[b_mT, b_Wout], writes=[b_ps])
                    cs = slice(cb * 512, (cb + 1) * 512)
                    P.op("dve", lambda e, x1=x1, ps=ps, cs=cs: e.tensor_tensor(out=x1[:, cs], in0=ps, in1=gA_b[:, cs], op=ALU.mult), reads=[b_ps, b_bc], writes=[b_x1])
                P.op("dve", lambda e, x1=x1, xt=xt: e.tensor_tensor(out=x1, in0=x1, in1=xt, op=ALU.add), reads=[b_x1, b_xt], writes=[b_x1])
                P.op("sp", lambda e, x1=x1, t=t: e.dma_start(out=X1s[t * 128:(t + 1) * 128, :], in_=x1), reads=[b_x1], writes=[b_X1s], dma=f"x1st{t % 2}")
                x1s[t] = (x1, b_x1)

            def s3(t):
                x1, b_x1 = x1s.pop(t)
                ss, b_ss = SS.next(); xs2, b_xs2 = XS2.next(); h2b, b_h2b = H2B.next(); h2T, b_h2T = H2T.next()
                junk, b_junk = JK.next()
                P.op("act", lambda e, junk=junk, x1=x1, ss=ss: e.activation(out=junk, in_=x1, func=AF.Square, accum_out=ss), reads=[b_x1], writes=[b_junk, b_ss])
                P.op("act", lambda e, ss=ss: e.activation(out=ss, in_=ss, func=AF.Ln, scale=1.0 / D, bias=EPS), reads=[b_ss], writes=[b_ss])
                P.op("act", lambda e, ss=ss: e.activation(out=ss, in_=ss, func=AF.Exp, scale=-0.5), reads=[b_ss], writes=[b_ss])
                P.op("dve", lambda e, xs2=xs2, x1=x1, ss=ss: e.scalar_tensor_tensor(out=xs2, in0=x1, scalar=ss, in1=GmB, op0=ALU.mult, op1=ALU.mult),
                     reads=[b_x1, b_ss, b_bc], writes=[b_xs2])
                P.op("dve", lambda e, xs2=xs2: e.tensor_tensor(out=xs2, in0=xs2, in1=SHmB, op=ALU.add), reads=[b_xs2, b_bc], writes=[b_xs2])
                P.op("act", lambda e, h2b=h2b, xs2=xs2: e.activation(out=h2b, in_=xs2, func=AF.Copy), reads=[b_xs2], writes=[b_h2b])
                for half in range(2):
                    tp, b_tp = bank(0)
                    tpv = tp.rearrange("p (k t) -> p k t", t=128)

                    def htr(e, tpv=tpv, xs2=xs2, half=half):
                        ins = None
                        for k in range(4):
                            kk = half * 4 + k
                            ins = e.transpose(out=tpv[:, k, :], in_=xs2[:, kk * 128:(kk + 1) * 128], identity=ident32)
                        return ins
                    P.op("pe", htr, reads=[b_xs2, b_ident32], writes=[b_tp])
                    P.op("act" if half == 0 else "dve",
                         (lambda e, h2T=h2T, tpv=tpv, half=half: e.activation(out=h2T[:, half * 4:(half + 1) * 4, :], in_=tpv, func=AF.Copy)) if half == 0 else
                         (lambda e, h2T=h2T, tpv=tpv, half=half: e.tensor_copy(out=h2T[:, half * 4:(half + 1) * 4, :], in_=tpv)),
                         reads=[b_tp], writes=[b_h2T])
                lp, b_lp = bank(0)
                lg, b_lg = LG.next(); m8, b_m8 = M8.next(); mk, b_mk = MK.next()
                ex, b_ex = EX.next(); sm, b_sm = SM.next()

                def mml(e, lp=lp, h2T=h2T):
                    ins = None
                    for k in range(8):
                        ins = e.matmul(lp[:, 0:NE], lhsT=h2T[:, k, :], rhs=rw32[:, k, :], start=(k == 0), stop=(k == 7))
                    return ins
                P.op("pe", mml, reads=[b_h2T, b_rw], writes=[b_lp])
                P.op("dve", lambda e, lg=lg, lp=lp: e.tensor_tensor(out=lg, in0=lp[:, 0:NE], in1=rb_b, op=ALU.add), reads=[b_lp, b_rbb], writes=[b_lg])
                P.op("dve", lambda e, m8=m8, lg=lg: e.max(out=m8, in_=lg), reads=[b_lg], writes=[b_m8])
                P.op("dve", lambda e, mk=mk, lg=lg, m8=m8: e.tensor_scalar(out=mk, in0=lg, scalar1=m8[:, 3:4], scalar2=None, op0=ALU.is_ge), reads=[b_lg, b_m8], writes=[b_mk])
                P.op("dve", lambda e, sm=sm, m8=m8: e.tensor_scalar(out=sm[:, 0:1], in0=m8[:, 0:1], scalar1=-1.0, scalar2=None, op0=ALU.mult), reads=[b_m8], writes=[b_sm])
                P.op("act", lambda e, ex=ex, lg=lg, sm=sm: e.activation(out=ex, in_=lg, func=AF.Exp, bias=sm[:, 0:1], scale=1.0), reads=[b_lg, b_sm], writes=[b_ex])
                P.op("dve", lambda e, ex=ex, mk=mk: e.tensor_tensor(out=ex, in0=ex, in1=mk, op=ALU.mult), reads=[b_ex, b_mk], writes=[b_ex])
                P.op("dve", lambda e, sm=sm, ex=ex: e.tensor_reduce(out=sm[:, 1:2], in_=ex, axis=AX.X, op=ALU.add), reads=[b_ex], writes=[b_sm])
                P.op("dve", lambda e, sm=sm: e.reciprocal(out=sm[:, 1:2], in_=sm[:, 1:2]), reads=[b_sm], writes=[b_sm])
                P.op("dve", lambda e, ex=ex, sm=sm, t=t: e.tensor_scalar(out=WdR[:, t, :], in0=ex, scalar1=sm[:, 1:2], scalar2=None, op0=ALU.mult), reads=[b_ex, b_sm], writes=[b_WdR[t]])
                P.op("sp", lambda e, h2b=h2b, t=t: e.dma_start(out=H2s[t * 128:(t + 1) * 128, :], in_=h2b), reads=[b_h2b], writes=[b_H2s], dma=f"h2st{t % 2}")

            prev3 = None
            for t in range(NOWN):
                if t + 1 < NOWN:
                    st1[t + 1] = stage1(t + 1)
                xt, b_xt, sg, b_sg = st1.pop(t)
                tr2 = P.record(lambda: s2(t, xt, b_xt, sg, b_sg))
                P.replay(([(prev3, [])] if prev3 is not None else []) + [(tr2, [])], width=2)
                prev3 = P.record(lambda: s3(t))
            P.replay([(prev3, [])], width=1)
            P.barrier()
            P.emit()

        if STOP_AFTER <= 3:
            return nc
        ph4 = contextlib.ExitStack()
        with ph4:
            WS = [(sb(ph4, f"ws{i}", [128, 8, D], BF16), (Buf(f"ws{i}a"), Buf(f"ws{i}b"))) for i in range(4)]
            bg = sb(ph4, "bg", [128, NE, 8], F32); bu1 = sb(ph4, "bu1", [128, NE, 8], F32); b_bgu = Buf("bgu")
            P.dma_group("b4", [("sp", lambda e: e.dma_start(out=bg.rearrange("p e k -> p (e k)"), in_=bg_l), [b_bgu]),
                               ("sp", lambda e: e.dma_start(out=bu1.rearrange("p e k -> p (e k)"), in_=bu_l), [b_bgu])])
            P.op("dve", lambda e: e.tensor_scalar(out=bu1, in0=bu1, scalar1=1.0, scalar2=None, op0=ALU.add), reads=[b_bgu], writes=[b_bgu])
            bdn = sb(ph4, "bdn", [NE, D], F32); b_bdn = Buf("bdn")
            P.op("sp", lambda e: e.dma_start(out=bdn, in_=b_down), writes=[b_bdn], dma="bdn")
            HR = sb(ph4, "hr", [128, 8, D], BF16); b_HR = Buf("hr")
            XTb = sb(ph4, "xtb", [128, 8, 1024], BF16); b_XTb = Buf("xtb")
            ACTT = mkring(ph4, "actT", 2, [128, 8, 512], BF16)
            G1 = mkring(ph4, "g1", 3, [128, 512], F32)
            TS = mkring(ph4, "ts", 2, [128, 512], F32)
            U0 = mkring(ph4, "u0", 2, [128, 512], F32)
            X1 = mkring(ph4, "x1c", 1, [128, D], F32)
            WT = mkring(ph4, "wT", 1, [NE, 128], F32)
            accA = yA.rearrange("p a b -> p (a b)").bitcast(F32).rearrange("p (i d) -> p i d", d=D)
            accB = yB.rearrange("p a b -> p (a b)").bitcast(F32).rearrange("p (i d) -> p i d", d=D)
            acc = [(accA[:, i, :] if i < 4 else accB[:, i - 4, :], Buf(f"acc{i}")) for i in range(8)]
            mats = (w_gate, w_up, w_down)
            NMAT = 2 * NE * 3

            def load_w(j):
                e_, m_ = (j // 3) % NE, j % 3
                w, b_w = WS[j % 4]
                src = mats[m_][e_].rearrange("(k p) n -> p k n", p=128)
                for h in range(2):
                    P.op("pool", lambda e, w=w, src=src, h=h: e.dma_start(out=w[:, h * 4:(h + 1) * 4, :], in_=src[:, h * 4:(h + 1) * 4, :]),
                         writes=[b_w[h]], dma=f"ws{j % 4}_{h}")
            for j in range(4):
                load_w(j)
            nload = 4
            PG = Ring([bank(0), bank(1)])
            PU = Ring([bank(2), bank(3)])
            PY = Ring([bank(4), bank(5)])
            PT = Ring([bank(6), bank(7)])
            for blk in range(2):
                P.op("sp", lambda e, blk=blk: e.dma_start(out=HR, in_=H2s[blk * 1024:(blk + 1) * 1024, :].rearrange("(n p) d -> p n d", p=128)),
                     reads=[b_H2s], writes=[b_HR], dma="hr")
                for n in range(8):
                    tp, b_tp = PT.next()
                    tpb = tp.bitcast(BF16).rearrange("p (k t) -> p k t", t=128)

                    def xtr(e, tpb=tpb, n=n):
                        ins = None
                        for k in range(8):
                            ins = e.transpose(out=tpb[:, k, :], in_=HR[:, n, k * 128:(k + 1) * 128], identity=ident_bf)
                        return ins
                    P.op("pe", xtr, reads=[b_HR, b_identbf], writes=[b_tp])
                    P.op("dve" if n % 2 == 0 else "act",
                         (lambda e, tpb=tpb, n=n: e.tensor_copy(out=XTb[:, :, n * 128:(n + 1) * 128], in_=tpb)) if n % 2 == 0 else
                         (lambda e, tpb=tpb, n=n: e.activation(out=XTb[:, :, n * 128:(n + 1) * 128], in_=tpb, func=AF.Copy)),
                         reads=[b_tp], writes=[b_XTb])
                for ex_ in range(NE):
                    jg = (blk * NE + ex_) * 3
                    Wg, b_Wg = WS[jg % 4]; Wu, b_Wu = WS[(jg + 1) % 4]; Wdn, b_Wdn = WS[(jg + 2) % 4]
                    for half in range(2):
                        xT = XTb[:, :, half * 512:(half + 1) * 512]
                        actT, b_actT = ACTT.next()
                        for j in range(8):
                            pg, b_pg = PG.next(); pu, b_pu = PU.next()

                            def mg_(e, pg=pg, j=j, Wg=Wg, xT=xT):
                                ins = None
                                for k in range(8):
                                    ins = e.matmul(pg, lhsT=Wg[:, k, j * 128:(j + 1) * 128], rhs=xT[:, k, :], start=(k == 0), stop=(k == 7))
                                return ins
                            P.op("pe", mg_, reads=[*b_Wg, b_XTb], writes=[b_pg])

                            def mu_(e, pu=pu, j=j, Wu=Wu, xT=xT):
                                ins = None
                                for k in range(8):
                                    ins = e.matmul(pu, lhsT=Wu[:, k, j * 128:(j + 1) * 128], rhs=xT[:, k, :], start=(k == 0), stop=(k == 7))
                                return ins
                            P.op("pe", mu_, reads=[*b_Wu, b_XTb], writes=[b_pu])
                            g1, b_g1 = G1.next(); ts, b_ts = TS.next(); u0, b_u0 = U0.next()
                            P.op("dve", lambda e, g1=g1, pg=pg, ex_=ex_, j=j: e.tensor_scalar(out=g1, in0=pg, scalar1=bg[:, ex_, j:j + 1], scalar2=7.0, op0=ALU.add, op1=ALU.min),
                                 reads=[b_pg, b_bgu], writes=[b_g1])
                            P.op("act", lambda e, ts=ts, g1=g1: e.activation(out=ts, in_=g1, func=AF.Silu, scale=1.702), reads=[b_g1], writes=[b_ts])
                            P.op("act", lambda e, u0=u0, pu=pu, ex_=ex_, j=j: e.activation(out=u0, in_=pu, func=AF.Identity, bias=bu1[:, ex_, j:j + 1], scale=1.0),
                                 reads=[b_pu, b_bgu], writes=[b_u0])
                            P.op("dve", lambda e, u0=u0: e.tensor_scalar(out=u0, in0=u0, scalar1=-6.0, scalar2=8.0, op0=ALU.max, op1=ALU.min), reads=[b_u0], writes=[b_u0])
                            P.op("dve", lambda e, actT=actT, u0=u0, ts=ts, j=j: e.scalar_tensor_tensor(out=actT[:, j, :], in0=u0, scalar=1.0 / 1.702, in1=ts, op0=ALU.mult, op1=ALU.mult),
                                 reads=[b_u0, b_ts], writes=[b_actT])
                        if half == 1:
                            while nload < min(NMAT, jg + 6):
                                load_w(nload); nload += 1
                        for n in range(4):
                            i = half * 4 + n
                            t = blk * 8 + i
                            a_ap, b_a = acc[i]
                            wsc = WdR[:, t, ex_:ex_ + 1]
                            for cb in range(2):
                                py, b_py = PY.next()
                                cs = slice(cb * 512, (cb + 1) * 512)

                                def md_(e, py=py, n=n, cb=cb, Wdn=Wdn, actT=actT):
                                    ins = None
                                    for j in range(8):
                                        ins = e.matmul(py, lhsT=actT[:, j, n * 128:(n + 1) * 128], rhs=Wdn[:, j, cb * 512:(cb + 1) * 512], start=(j == 0), stop=(j == 7))
                                    return ins
                                P.op("pe", md_, reads=[*b_Wdn, b_actT], writes=[b_py])
                                if ex_ == 0:
                                    P.op("dve", lambda e, a_ap=a_ap, py=py, wsc=wsc, cs=cs: e.tensor_scalar(out=a_ap[:, cs], in0=py, scalar1=wsc, scalar2=None, op0=ALU.mult),
                                         reads=[b_py, b_WdR[t]], writes=[b_a])
                                else:
                                    P.op("dve", lambda e, a_ap=a_ap, py=py, wsc=wsc, cs=cs: e.scalar_tensor_tensor(out=a_ap[:, cs], in0=py, scalar=wsc, in1=a_ap[:, cs], op0=ALU.mult, op1=ALU.add),
                                         reads=[b_py, b_WdR[t], b_a], writes=[b_a])
                    while nload < min(NMAT, jg + 7):
                        load_w(nload); nload += 1
                for i in range(8):
                    t = blk * 8 + i
                    a_ap, b_a = acc[i]
                    x1, b_x1 = X1.next()
                    P.op("sp", lambda e, x1=x1, t=t: e.dma_start(out=x1, in_=X1s[t * 128:(t + 1) * 128, :]), reads=[b_X1s], writes=[b_x1], dma="x1c")
                    wT, b_wT = WT.next()
                    tp, b_tp = PT.next()
                    P.op("pe", lambda e, tp=tp, t=t: e.transpose(out=tp[0:NE, 0:128], in_=WdR[:, t, :], identity=ident32), reads=[b_WdR[t], b_ident32], writes=[b_tp])
                    P.op("act", lambda e, wT=wT, tp=tp: e.activation(out=wT, in_=tp[0:NE, 0:128], func=AF.Copy), reads=[b_tp], writes=[b_wT])
                    for cb in range(2):
                        pbd, b_pbd = PY.next()
                        cs = slice(cb * 512, (cb + 1) * 512)
                        P.op("pe", lambda e, pbd=pbd, wT=wT, cs=cs: e.matmul(pbd, lhsT=wT, rhs=bdn[:, cs], start=True, stop=True), reads=[b_wT, b_bdn], writes=[b_pbd])
                        P.op("dve", lambda e, a_ap=a_ap, pbd=pbd, cs=cs: e.tensor_tensor(out=a_ap[:, cs], in0=pbd, in1=a_ap[:, cs], op=ALU.add), reads=[b_pbd, b_a], writes=[b_a])
                    P.op("dve", lambda e, a_ap=a_ap: e.tensor_tensor(out=a_ap, in0=a_ap, in1=gM_b, op=ALU.mult), reads=[b_a, b_bc], writes=[b_a])
                    P.op("dve", lambda e, a_ap=a_ap, x1=x1: e.tensor_tensor(out=a_ap, in0=a_ap, in1=x1, op=ALU.add), reads=[b_a, b_x1], writes=[b_a])
                    P.op("sp", lambda e, a_ap=a_ap, t=t: e.dma_start(out=out_d[t * 128:(t + 1) * 128, :], in_=a_ap), reads=[b_a], dma=f"ost{i}")
            P.barrier()
            P.emit()
    return nc


def _rope_tables():
    rows = SEQ // 64
    row = np.repeat(np.arange(rows, dtype=np.float32), 64)
    col = np.tile(np.arange(64, dtype=np.float32), rows)
    inv = (1.0 / (10000.0 ** (np.arange(16, dtype=np.float32) / 16))).astype(np.float32)
    ang = np.concatenate([row[:, None] * inv, col[:, None] * inv], axis=-1).astype(np.float32)
    cos, sin = np.cos(ang).astype(np.float32), np.sin(ang).astype(np.float32)
    tab = np.zeros((SEQ, 128), np.float32)
    tab[:, 0:64:2] = cos
    tab[:, 1:64:2] = cos
    tab[:, 64:128:2] = -sin
    tab[:, 65:128:2] = sin
    return tab


def _chunk_layout(v):
    return np.ascontiguousarray(v.reshape(-1, 128).T)


def make_in_maps(inputs):
    f = lambda a: np.ascontiguousarray(np.asarray(a, dtype=np.float32))
    x = f(inputs["x"]); c = f(inputs["c"]); ctx = f(inputs["ctx"]); c_ctx = f(inputs["c_ctx"])
    rope = _rope_tables()
    shared = {
        "w_ada": f(inputs["w_ada"][0]),
        "b_ada_l": _chunk_layout(f(inputs["b_ada"][0])),
        "nrm_a_l": _chunk_layout(f(inputs["norm_attn"][0])),
        "nrm_m_l": _chunk_layout(f(inputs["norm_mlp"][0])),
        "w_in": f(inputs["w_in"][0]),
        "qk_gain": np.stack([f(inputs[k][0]) for k in ("q_norm_a", "k_norm_a", "q_norm_b", "k_norm_b")]),
        "lam_v": np.stack([f(inputs[k][0]) for k in ("lambda_q1", "lambda_k1", "lambda_q2", "lambda_k2")]),
        "subln": f(inputs["subln_b"][0]),
        "w_oa": f(inputs["w_oa"][0]), "w_ob": f(inputs["w_ob"][0]), "w_out": f(inputs["w_out"][0]),
        "router_w": f(inputs["router_w"][0]), "router_b": f(inputs["router_b"][0]),
        "w_gate": f(inputs["w_gate"][0]), "w_up": f(inputs["w_up"][0]), "w_down": f(inputs["w_down"][0]),
        "bg_l": np.ascontiguousarray(f(inputs["b_gate"][0]).reshape(NE, 8, 128).transpose(2, 0, 1).reshape(128, NE * 8)),
        "bu_l": np.ascontiguousarray(f(inputs["b_up"][0]).reshape(NE, 8, 128).transpose(2, 0, 1).reshape(128, NE * 8)),
        "b_down": f(inputs["b_down"][0]),
    }
    maps = []
    for i in range(NCORES):
        b, j = i // 4, i % 4
        order = np.concatenate([np.arange(j * OWN, (j + 1) * OWN), np.arange(0, j * OWN), np.arange((j + 1) * OWN, SEQ)])
        cvec = np.empty((128, 16), np.float32)
        cvec[:, 0::2] = _chunk_layout(c[b])
        cvec[:, 1::2] = _chunk_layout(c_ctx)
        m = dict(shared)
        m["xp"] = np.ascontiguousarray(x[b][order])
        m["ctx"] = ctx[b]
        m["rope"] = np.ascontiguousarray(rope[order])
        m["cvec"] = cvec
        maps.append(m)
    return maps


_NC_CACHE = {}


def kernel(**inputs):
    if "nc" not in _NC_CACHE:
        _NC_CACHE["nc"] = build_program()
    nc = _NC_CACHE["nc"]
    in_maps = make_in_maps(inputs)
    res = run_bass_kernel_spmd(nc, in_maps, core_ids=list(range(NCORES)))
    out = np.empty((2, SEQ, D), np.float32)
    for i in range(NCORES):
        b, j = i // 4, i % 4
        out[b, j * OWN:(j + 1) * OWN] = res.results[i]["out"]
    if DEBUG:
        _NC_CACHE["res"] = res
    return out
```
